# Optimizing a Trainium2 kernel written in Bass

```python
import jax
import jax.numpy as jnp
from jax import lax
import numpy as np


D_MODEL = 2048
BATCH = 1
SEQ = 16384
DEPTH = 1

HEAD_DIM = 128
ATTN_WIDTH = 3 * D_MODEL // 4
N_ATTN_HEADS = ATTN_WIDTH // HEAD_DIM
ATTN_GROUPS = ((128, 1), (512, 4), (2048, 16))
N_GROUPS = len(ATTN_GROUPS)
HEADS_PER_GROUP = N_ATTN_HEADS // N_GROUPS
ATTN_OUT_WIDTH = HEADS_PER_GROUP * HEAD_DIM
BAND = 128
ROPE_THETA = 10000.0
POOL_WIDTH = D_MODEL // 4
POOL_WINDOWS = (2, 4, 8, 16)
POOL_GROUP = POOL_WIDTH // len(POOL_WINDOWS)
IN_WIDTH = 3 * ATTN_WIDTH + POOL_WIDTH + 2 * D_MODEL
PEER_HEADS = 8
PEER_QUERY_DIM = 256
PEER_HALF = PEER_QUERY_DIM // 2
PEER_N_KEYS = 128
PEER_N_EXPERTS = PEER_N_KEYS * PEER_N_KEYS
PEER_TOPK = 16
PEER_CHUNK = 128
EPS = 1e-6

kernel_name = 'hybrid_dilated_pool_peer_block'


def rms_norm(x, g):
    x32 = x.astype(jnp.float32)
    y = x32 * lax.rsqrt(jnp.mean(x32 * x32, axis=-1, keepdims=True) + EPS)
    return y.astype(x.dtype) * g


def apply_rope(t, pos):
    half = t.shape[-1] // 2
    inv = jnp.power(jnp.float32(ROPE_THETA), -jnp.arange(half, dtype=jnp.float32) / half)
    ang = pos[:, None] * inv[None, :]
    cos = jnp.cos(ang)[None, :, None, :]
    sin = jnp.sin(ang)[None, :, None, :]
    t32 = t.astype(jnp.float32)
    t1, t2 = t32[..., :half], t32[..., half:]
    return jnp.concatenate([t1 * cos - t2 * sin, t2 * cos + t1 * sin], axis=-1).astype(t.dtype)


def dilated_window_attention(q, k, v, dil, steps):
    B, S, H, Dh = q.shape
    L = S // dil
    nb = -(-L // BAND)
    Lp = nb * BAND

    def to_sub(t):
        t = t.reshape(B, L, dil, H, Dh).transpose(0, 2, 1, 3, 4)
        return jnp.pad(t, ((0, 0), (0, 0), (0, Lp - L), (0, 0), (0, 0)))

    def band(t):
        t = jnp.pad(t, ((0, 0), (0, 0), (BAND, 0), (0, 0), (0, 0))).reshape(B, dil, nb + 1, BAND, H, Dh)
        return jnp.concatenate([t[:, :, :-1], t[:, :, 1:]], axis=3)

    qb = to_sub(q).reshape(B, dil, nb, BAND, H, Dh)
    kb = band(to_sub(k))
    vb = band(to_sub(v))
    s = jnp.einsum('brnqhd,brnkhd->brnhqk', qb, kb).astype(jnp.float32) * (Dh ** -0.5)
    qi = jnp.arange(BAND)[:, None]
    kj = jnp.arange(2 * BAND)[None, :]
    dist = qi + BAND - kj
    key_idx = jnp.arange(nb)[:, None, None] * BAND + kj[None] - BAND
    valid = (dist[None] >= 0) & (dist[None] <= steps) & (key_idx >= 0)
    s = jnp.where(valid[None, None, :, None], s, -jnp.inf)
    m = jnp.max(s, axis=-1, keepdims=True)
    p = jnp.exp(s - m)
    denom = jnp.sum(p, axis=-1, keepdims=True)
    o = jnp.einsum('brnhqk,brnkhd->brnqhd', (p / denom).astype(v.dtype), vb)
    lse = (m + jnp.log(denom))[..., 0]
    o = o.reshape(B, dil, Lp, H, Dh)[:, :, :L].transpose(0, 2, 1, 3, 4).reshape(B, S, H, Dh)
    lse = lse.transpose(0, 1, 2, 4, 3).reshape(B, dil, Lp, H)[:, :, :L]
    lse = lse.transpose(0, 2, 1, 3).reshape(B, S, H)
    return o, lse


def dilated_mixture_attention(q, k, v):
    B, S, _, Dh = q.shape
    outs, lses = [], []
    for gi, (window, dil) in enumerate(ATTN_GROUPS):
        hs = slice(gi * HEADS_PER_GROUP, (gi + 1) * HEADS_PER_GROUP)
        o, l = dilated_window_attention(q[:, :, hs], k[:, :, hs], v[:, :, hs], dil, window // dil)
        outs.append(o)
        lses.append(l)
    o = jnp.stack(outs, axis=0).astype(jnp.float32)
    w = jax.nn.softmax(jnp.stack(lses, axis=0), axis=0)[..., None]
    y = jnp.sum(w * o, axis=0).astype(q.dtype)
    return y.reshape(B, S, ATTN_OUT_WIDTH)


def multiscale_pool(p, mix, scale):
    B, S, C = p.shape
    p32 = p.astype(jnp.float32)
    cs = jnp.concatenate([jnp.zeros((B, 1, C), jnp.float32), lax.cumsum(p32, axis=1)], axis=1)
    t = jnp.arange(S)
    outs = []
    for g, w in enumerate(POOL_WINDOWS):
        sl = slice(g * POOL_GROUP, (g + 1) * POOL_GROUP)
        c_g = cs[..., sl]
        lo = jnp.concatenate([jnp.zeros((B, w - 1, POOL_GROUP), jnp.float32), c_g[:, :S - w + 1]], axis=1)
        mean = (c_g[:, 1:] - lo) / jnp.minimum(t + 1, w).astype(jnp.float32)[None, :, None]
        outs.append(mean - p32[..., sl])
    d = jnp.stack(outs, axis=2).astype(p.dtype)
    y = jnp.einsum('bsgc,gcd->bsgd', d, mix).reshape(B, S, C)
    return y * scale


def peer_ffn(xn, w_query, sub_keys, w_down, w_up):
    B, S, D = xn.shape
    q = jnp.einsum('bsd,de->bse', xn, w_query).reshape(B, S, PEER_HEADS, 2, PEER_HALF)
    s = jnp.einsum('bshpd,hpnd->bshpn', q, sub_keys).astype(jnp.float32)
    top_s, top_i = lax.top_k(s, PEER_TOPK)
    cand = top_s[..., 0, :, None] + top_s[..., 1, None, :]
    cand_s, cand_i = lax.top_k(cand.reshape(B, S, PEER_HEADS, PEER_TOPK * PEER_TOPK), PEER_TOPK)
    i1 = jnp.take_along_axis(top_i[..., 0, :], cand_i // PEER_TOPK, axis=-1)
    i2 = jnp.take_along_axis(top_i[..., 1, :], cand_i % PEER_TOPK, axis=-1)
    expert = i1 * PEER_N_KEYS + i2
    gates = jax.nn.softmax(cand_s, axis=-1).astype(xn.dtype)
    n_chunk = (B * S) // PEER_CHUNK
    xs = xn.reshape(n_chunk, PEER_CHUNK, D)
    ids = expert.reshape(n_chunk, PEER_CHUNK, PEER_HEADS * PEER_TOPK)
    gs = gates.reshape(n_chunk, PEER_CHUNK, PEER_HEADS * PEER_TOPK)

    def one_block(args):
        xc, ic, gc = args
        u = jnp.take(w_down, ic, axis=0)
        a = jax.nn.gelu(jnp.einsum('cd,ced->ce', xc, u), approximate=False) * gc
        vv = jnp.take(w_up, ic, axis=0)
        return jnp.einsum('ce,ced->cd', a, vv)

    out = lax.map(one_block, (xs, ids, gs))
    return out.reshape(B, S, D)


def setup_inputs(seed: int = 0) -> dict:
    key = jax.random.key(seed)
    ks = jax.random.split(key, 17)
    f32 = jnp.float32

    def nrm(k, shape, scale):
        return jax.random.normal(k, shape, f32) * scale

    n_pool = len(POOL_WINDOWS)
    return {
        'x': nrm(ks[0], (BATCH, SEQ, D_MODEL), 1.0),
        'c': nrm(ks[1], (BATCH, D_MODEL), 1.0),
        'ada_w': nrm(ks[2], (DEPTH, D_MODEL, 6 * D_MODEL), 0.5 * D_MODEL ** -0.5),
        'ada_b': nrm(ks[3], (DEPTH, 6 * D_MODEL), 0.02),
        'norm1_g': 1.0 + nrm(ks[4], (DEPTH, D_MODEL), 0.02),
        'w_in': nrm(ks[5], (DEPTH, D_MODEL, IN_WIDTH), D_MODEL ** -0.5),
        'pool_mix': nrm(ks[6], (DEPTH, n_pool, POOL_GROUP, POOL_GROUP), POOL_GROUP ** -0.5),
        'pool_scale': 1.0 + nrm(ks[7], (DEPTH, POOL_WIDTH), 0.1),
        'w_attn_branch': nrm(ks[8], (DEPTH, ATTN_OUT_WIDTH, D_MODEL), ATTN_OUT_WIDTH ** -0.5),
        'w_pool_branch': nrm(ks[9], (DEPTH, POOL_WIDTH, D_MODEL), POOL_WIDTH ** -0.5),
        'w_out': nrm(ks[10], (DEPTH, D_MODEL, D_MODEL), D_MODEL ** -0.5),
        'norm2_g': 1.0 + nrm(ks[11], (DEPTH, D_MODEL), 0.02),
        'peer_wq': nrm(ks[12], (DEPTH, D_MODEL, PEER_HEADS * PEER_QUERY_DIM), D_MODEL ** -0.5),
        'peer_keys': nrm(ks[13], (DEPTH, PEER_HEADS, 2, PEER_N_KEYS, PEER_HALF), PEER_HALF ** -0.5),
        'peer_down': nrm(ks[14], (DEPTH, PEER_N_EXPERTS, D_MODEL), D_MODEL ** -0.5),
        'peer_up': nrm(ks[15], (DEPTH, PEER_N_EXPERTS, D_MODEL), PEER_HEADS ** -0.5),
        'final_g': 1.0 + nrm(ks[16], (D_MODEL,), 0.02),
    }


def reference(x, c, ada_w, ada_b, norm1_g, w_in, pool_mix, pool_scale, w_attn_branch, w_pool_branch, w_out, norm2_g, peer_wq, peer_keys, peer_down, peer_up, final_g):
    B, S, D = x.shape
    pos = jnp.arange(S, dtype=jnp.float32)
    h = x
    for layer in range(DEPTH):
        mod = jax.nn.silu(c) @ ada_w[layer] + ada_b[layer]
        shift1, scale1, gate1, shift2, scale2, gate2 = jnp.split(mod[:, None, :], 6, axis=-1)

        xn = rms_norm(h, norm1_g[layer]) * (1.0 + scale1) + shift1
        proj = xn @ w_in[layer]
        q = proj[..., :ATTN_WIDTH].reshape(B, S, N_ATTN_HEADS, HEAD_DIM)
        k = proj[..., ATTN_WIDTH:2 * ATTN_WIDTH].reshape(B, S, N_ATTN_HEADS, HEAD_DIM)
        v = proj[..., 2 * ATTN_WIDTH:3 * ATTN_WIDTH].reshape(B, S, N_ATTN_HEADS, HEAD_DIM)
        off = 3 * ATTN_WIDTH
        p_in = proj[..., off:off + POOL_WIDTH]
        gate_a = jax.nn.sigmoid(proj[..., off + POOL_WIDTH:off + POOL_WIDTH + D])
        gate_b = jax.nn.sigmoid(proj[..., off + POOL_WIDTH + D:])
        q = apply_rope(q, pos)
        k = apply_rope(k, pos)
        attn = dilated_mixture_attention(q, k, v)
        pool = multiscale_pool(p_in, pool_mix[layer], pool_scale[layer])
        merged = gate_a * (attn @ w_attn_branch[layer]) + gate_b * (pool @ w_pool_branch[layer])
        h = h + gate1 * (merged @ w_out[layer])

        xn2 = rms_norm(h, norm2_g[layer]) * (1.0 + scale2) + shift2
        h = h + gate2 * peer_ffn(xn2, peer_wq[layer], peer_keys[layer], peer_down[layer], peer_up[layer])
    return rms_norm(h, final_g)
```

```python
import numpy as np
from contextlib import ExitStack
import concourse.bass as bass
import concourse.mybir as mybir
from concourse.ap import AP
from concourse.bass_utils import run_bass_kernel_spmd

F32 = mybir.dt.float32
BF16 = mybir.dt.bfloat16
AF = mybir.ActivationFunctionType
ALU = mybir.AluOpType
AX = mybir.AxisListType
PE, ACT, DVE, POOL, SP = "pe", "act", "dve", "pool", "sp"
NCORES = 8


class _Op:
    __slots__ = ("eng", "fn", "deps", "dma", "semkey", "has_dep", "ticket", "idx")


def _is_ps(k):
    return isinstance(k, str) and k.startswith("ps")


class Prog:
    def __init__(self, nc):
        self.nc = nc
        self.ops = []
        self.last_w = {}
        self.readers = {}
        self.pending = {}
        self.last_on_eng = {}
        self.last_dma = {}

    def barrier(self):
        deps = set(self.last_on_eng.values()) | set(self.last_dma.values())
        for e in (PE, ACT, DVE, POOL, SP):
            self.pending[e] = set(deps)

    def op(self, eng, fn, reads=(), writes=(), dma=False, semkey=None):
        o = _Op()
        o.eng, o.fn, o.dma, o.has_dep, o.ticket = eng, fn, dma, False, None
        o.idx = len(self.ops)
        reads = list(reads)
        writes = list(writes)
        pr = [k for k in reads if _is_ps(k)]
        if pr:
            reads = [k for k in reads if not _is_ps(k)]
            writes = writes + pr
        deps = set()
        for k in reads:
            w = self.last_w.get(k)
            if w is not None:
                deps.add(w)
        for k in writes:
            w = self.last_w.get(k)
            if w is not None:
                deps.add(w)
            for r in self.readers.get(k, ()):
                deps.add(r)
        pend = self.pending.pop(eng, None)
        if pend:
            deps |= pend
        deps.discard(o.idx)
        o.deps = deps
        if dma:
            sk = semkey if semkey is not None else (writes[0] if writes else reads[0])
            o.semkey = (eng, sk)
            self.last_dma[o.semkey] = o.idx
        else:
            o.semkey = None
            self.last_on_eng[eng] = o.idx
        for k in reads:
            self.readers.setdefault(k, []).append(o.idx)
        for k in writes:
            self.last_w[k] = o.idx
            self.readers[k] = []
        self.ops.append(o)
        return o

    def emit(self):
        nc = self.nc
        ops = self.ops
        for o in ops:
            nd = set()
            for d in o.deps:
                p = ops[d]
                if (not p.dma) and (not o.dma) and p.eng == o.eng and o.eng == PE:
                    continue
                nd.add(d)
            o.deps = nd
            for d in nd:
                ops[d].has_dep = True
        with ExitStack() as es:
            engsem = {e: es.enter_context(nc.semaphore("s_" + e)) for e in (PE, ACT, DVE, POOL, SP)}
            dmasem = {}
            for o in ops:
                if o.dma and o.semkey not in dmasem:
                    dmasem[o.semkey] = es.enter_context(nc.semaphore("d%d" % len(dmasem)))
            cnt = {e: 0 for e in engsem}
            dcnt = {k: 0 for k in dmasem}
            for o in ops:
                if o.dma:
                    dcnt[o.semkey] += 16
                    o.ticket = dcnt[o.semkey]
                elif o.has_dep:
                    cnt[o.eng] += 1
                    o.ticket = cnt[o.eng]
            running = {k: 0 for k in dmasem}
            waits = []
            for o in ops:
                ws = {}
                for d in o.deps:
                    p = ops[d]
                    if p.dma:
                        key, val = ("d", p.semkey), running[p.semkey]
                    else:
                        key, val = ("e", p.eng), p.ticket
                    if ws.get(key, 0) < val:
                        ws[key] = val
                waits.append(ws)
                if o.dma:
                    running[o.semkey] = o.ticket
            final = dict(running)
            self.n_sems = len(engsem) + len(dmasem)
            engmap = {PE: "tensor", ACT: "scalar", DVE: "vector", POOL: "gpsimd", SP: "sync"}
            with nc.Block() as block:
                for e in (SP, POOL, ACT, DVE, PE):
                    myops = [o for o in ops if o.eng == e]

                    def body(eng, e=e, myops=myops):
                        waited = {}
                        for o in myops:
                            for key, val in waits[o.idx].items():
                                if waited.get(key, 0) >= val:
                                    continue
                                waited[key] = val
                                sem = dmasem[key[1]] if key[0] == "d" else engsem[key[1]]
                                eng.wait_ge(sem, val)
                            ins = o.fn(eng)
                            if o.dma:
                                ins.then_inc(dmasem[o.semkey], 16)
                            elif o.has_dep:
                                ins.then_inc(engsem[e], 1)
                        if e == SP:
                            for k, v in final.items():
                                if v > 0 and waited.get(("d", k), 0) < v:
                                    eng.wait_ge(dmasem[k], v)
                            for e2 in (PE, ACT, DVE, POOL):
                                if cnt[e2] > 0:
                                    eng.wait_ge(engsem[e2], cnt[e2])

                    getattr(block, engmap[e])(body)


def raw(ap2d, col_off, dims):
    return AP(ap2d.tensor, ap2d.offset + col_off, [list(ap2d.ap[0])] + [list(d) for d in dims])


DBG = {}


def build(debug=()):
    nc = bass.Bass("TRN2", target_bir_lowering=False)
    P = Prog(nc)

    def din(name, shape, dt=F32):
        return nc.dram_tensor(name, list(shape), dt, kind="ExternalInput").ap()

    def dscr(name, shape, dt):
        kind = "ExternalOutput" if name in debug else "Internal"
        return nc.dram_tensor(name, list(shape), dt, kind=kind).ap()

    xh = din("xh", [4096, 2048])
    cosT = din("cosT", [128, 4096])
    sinT = din("sinT", [128, 4096])
    c_in = din("c_in", [128, 16])
    ada_w = din("ada_w", [2048, 12288])
    ada_b = din("ada_b", [1, 12288])
    g1 = din("g1", [1, 2048])
    g2 = din("g2", [1, 2048])
    gf = din("gf", [1, 2048])
    g1T = din("g1T", [128, 16])
    g2T = din("g2T", [128, 16])
    w_in = din("w_in", [2048, 9216])
    pmix = din("pmix", [512, 128])
    pscale = din("pscale", [128, 4])
    wab = din("wab", [512, 2048])
    wpb = din("wpb", [512, 2048])
    w_out = din("w_out", [2048, 2048])
    wq = din("wq", [2048, 2048])
    keysT = din("keysT", [128, 2048])
    wdT = din("wdT", [2048, 16384])
    wup = din("wup", [16384, 2048])
    ident = din("ident", [128, 128])
    rotT = din("rotT", [128, 128])
    mprev = din("mprev", [128, 128])
    mcur = din("mcur", [128, 128])
    mfirst = din("mfirst", [128, 128])
    flag = din("flag", [128, 1])
    pcorr = din("pcorr", [128, 64])
    repm = din("repm", [24, 128])
    out = nc.dram_tensor("out", [2048, 2048], F32, kind="ExternalOutput").ap()

    mod_D = dscr("mod_D", [1, 12288], F32)
    xnT_D = dscr("xnT_D", [16, 128, 4096], BF16)
    attnT_D = dscr("attnT_D", [4, 128, 2048], BF16)
    mergedT_D = dscr("mergedT_D", [16, 128, 2048], BF16)
    h1_D = dscr("h1_D", [2048, 2048], F32)
    xn2T_D = dscr("xn2T_D", [16, 128, 2048], BF16)
    s_D = dscr("s_D", [16, 16, 128, 128], F32)
    G_D = dscr("G_D", [16, 128, 16384], BF16)
    s3_D = dscr("s3_D", [16, 3, 16, 128, 128], BF16)

    es = ExitStack()
    SLABF = 49152
    slab = es.enter_context(nc.sbuf_tensor("slab", [128, SLABF], F32))
    cst = es.enter_context(nc.sbuf_tensor("cst", [128, 1280], F32))
    psb = [es.enter_context(nc.psum_tensor("psb%d" % i, [128, 512], F32)) for i in range(8)]
    psk = ["ps%d" % i for i in range(8)]

    def psf(i):
        return psb[i][:]

    def psh(i):
        return psb[i][:].bitcast(BF16)

    class Arena:
        def __init__(self):
            self.off = 0

        def f(self, n):
            v = slab[:, self.off:self.off + n]
            self.off += n
            assert self.off <= SLABF, self.off
            return v

        def h(self, n):
            assert n % 2 == 0
            v = slab[:, self.off:self.off + n // 2].bitcast(BF16)
            self.off += n // 2
            assert self.off <= SLABF, self.off
            return v

    def mm(o, lhsT, rhs, start, stop, r, w):
        P.op(PE, lambda e: e.matmul(o, lhsT=lhsT, rhs=rhs, start=start, stop=stop), reads=r, writes=w)

    def tr(o, in_, idn, r, w):
        P.op(PE, lambda e: e.transpose(out=o, in_=in_, identity=idn), reads=r, writes=w)

    def act(o, in_, func, r, w, bias=None, scale=None, accum=None):
        kw = {}
        if bias is not None:
            kw["bias"] = bias
        if scale is not None:
            kw["scale"] = scale
        if accum is not None:
            kw["accum_out"] = accum
        P.op(ACT, lambda e: e.activation(out=o, in_=in_, func=func, **kw), reads=r, writes=w)

    def amul(o, in_, m, r, w):
        P.op(ACT, lambda e: e.mul(out=o, in_=in_, mul=m), reads=r, writes=w)

    def tt(eng, o, a, b, op, r, w):
        P.op(eng, lambda e: e.tensor_tensor(out=o, in0=a, in1=b, op=op), reads=r, writes=w)

    def stt(eng, o, a, s, b, op0, op1, r, w):
        P.op(eng, lambda e: e.scalar_tensor_tensor(out=o, in0=a, scalar=s, in1=b, op0=op0, op1=op1), reads=r, writes=w)

    def tsc(eng, o, a, s1, op0, r, w):
        P.op(eng, lambda e: e.tensor_scalar(out=o, in0=a, scalar1=s1, scalar2=None, op0=op0), reads=r, writes=w)

    def cp(eng, o, in_, r, w):
        if eng == ACT:
            P.op(ACT, lambda e: e.activation(out=o, in_=in_, func=AF.Copy), reads=r, writes=w)
        else:
            P.op(eng, lambda e: e.tensor_copy(out=o, in_=in_), reads=r, writes=w)

    def dma(eng, o, in_, r, w, semkey=None):
        P.op(eng, lambda e: e.dma_start(out=o, in_=in_), reads=r, writes=w, dma=True, semkey=semkey)

    def recip(o, in_, r, w):
        P.op(DVE, lambda e: e.reciprocal(out=o, in_=in_), reads=r, writes=w)

    def vmax(o, in_, r, w):
        P.op(DVE, lambda e: e.max(out=o, in_=in_), reads=r, writes=w)

    def vmr(o, rep, vals, r, w):
        P.op(DVE, lambda e: e.match_replace(out=o, in_to_replace=rep, in_values=vals, imm_value=-1e30), reads=r, writes=w)

    def memset(eng, o, val, w):
        P.op(eng, lambda e: e.memset(o, val), reads=[], writes=w)

    id_f = cst[:, 0:128]
    rot_f = cst[:, 128:256]
    id_b = cst[:, 256:320].bitcast(BF16)
    mp_b = cst[:, 320:448].bitcast(BF16)
    mf_b = cst[:, 448:576].bitcast(BF16)
    ones_b = cst[:, 576:640].bitcast(BF16)
    flag_s = cst[:, 640:641]
    pcorr_s = cst[:, 704:768]
    pscale_s = cst[:, 768:772]
    csil = cst[:, 800:816]
    craw = cst[:, 816:832]
    rep_b = cst[0:24, 832:896].bitcast(BF16)
    dma(SP, id_f, ident, [], ["id_f"])
    dma(SP, rot_f, rotT, [], ["rot_f"])
    dma(POOL, id_b, ident, [], ["id_b"])
    rot_b = cst[:, 1130:1194].bitcast(BF16)
    dma(POOL, rot_b, rotT, [], ["rot_b"])
    dma(POOL, mp_b[:, 0:128], mprev, [], ["mp_b"], semkey="mp_b")
    dma(POOL, mp_b[:, 128:256], mcur, [], ["mp_b"], semkey="mp_b")
    dma(POOL, mf_b[:, 0:128], mfirst, [], ["mf_b"], semkey="mf_b")
    dma(POOL, mf_b[:, 128:256], mcur, [], ["mf_b"], semkey="mf_b")
    dma(SP, flag_s, flag, [], ["flag"])
    dma(SP, pcorr_s, pcorr, [], ["pcorr"])
    dma(SP, pscale_s, pscale, [], ["pscale"])
    dma(SP, craw, c_in, [], ["craw"])
    modT = cst[:, 960:1056]
    one_f = cst[0:1, 1056:1057]
    g1T_s = cst[:, 1060:1076]
    g2T_s = cst[:, 1076:1092]
    geff1T = cst[:, 1092:1108]
    geff2T = cst[:, 1108:1124]
    memset(DVE, one_f, 1.0, ["one_f"])
    dma(SP, g1T_s, g1T, [], ["g1T_s"])
    dma(SP, g2T_s, g2T, [], ["g2T_s"])
    dma(POOL, rep_b, repm, [], ["rep_b"])
    memset(DVE, ones_b, 1.0, ["ones_b"])
    act(csil, craw, AF.Silu, ["craw"], ["csil"])

    def bcast_row(dst, src_row, key, rd=()):
        dma(SP, dst, src_row.partition_broadcast(128), list(rd), [key])

    def norm_mod_T(xt, kx, geffT, shT, kaff, junk, xs, stat, dstv, kdst, banks, tag, stage=0):
        ss, rt, rstd = stat[:, 0:1], stat[:, 1:2], stat[:, 2:3]
        b0, b1 = banks
        if stage in (0, 1):
            act(junk, xt, AF.Square, [kx], [tag + "junk", tag + "ss"], accum=ss)
            act(rt, ss, AF.Sqrt, [tag + "ss"], [tag + "rt"], bias=1e-6, scale=1.0 / 2048)
            recip(rstd, rt, [tag + "rt"], [tag + "rstd"])
            tsc(DVE, xs, xt, rstd, ALU.mult, [kx, tag + "rstd"], [tag + "xs"])
            for k in range(16):
                b = b0 if k < 8 else b1
                tr(psh(b)[:, (k % 8) * 128:(k % 8 + 1) * 128], xs[:, k * 128:(k + 1) * 128], id_b,
                   [tag + "xs", "id_b"], [psk[b]])
        if stage == 1:
            return
        for k in range(16):
            b = b0 if k < 8 else b1
            src = psh(b)[:, (k % 8) * 128:(k % 8 + 1) * 128]
            if k < 8:
                act(dstv[:, k, :], src, AF.Identity, [psk[b]] + kaff, [kdst], bias=shT[:, k:k + 1], scale=geffT[:, k:k + 1])
            else:
                P.op(DVE, lambda e, o=dstv[:, k, :], a=src, s1_=geffT[:, k:k + 1], s2_=shT[:, k:k + 1]:
                     e.tensor_scalar(out=o, in0=a, scalar1=s1_, scalar2=s2_, op0=ALU.mult, op1=ALU.add),
                     reads=[psk[b]] + kaff, writes=[kdst])

    A = Arena()
    NAW = 4
    aw = [A.h(16 * 512).rearrange("p (k m) -> p k m", k=16) for _ in range(NAW)]
    abt = [A.f(512) for _ in range(2)]
    mrow = [A.f(512) for _ in range(2)]
    csil_b = A.h(16)
    cp(DVE, csil_b, csil, ["csil"], ["csil_b"])

    def ada_tile(n):
        s = n % 2
        sw = n % NAW
        dma(POOL, aw[sw], ada_w[:, n * 512:(n + 1) * 512].rearrange("(k p) m -> p k m", p=128), [], ["aw%d" % sw])
        dma(SP, abt[s][0:1, :], ada_b[0:1, n * 512:(n + 1) * 512], [], ["abt%d" % s])
        for k in range(16):
            mm(psf(6 + s)[0:1, :], csil_b[:, k:k + 1], aw[sw][:, k, :], k == 0, k == 15, ["csil_b", "aw%d" % sw], [psk[6 + s]])
        tt(DVE, mrow[s][0:1, :], psf(6 + s)[0:1, :], abt[s][0:1, :], ALU.add, [psk[6 + s], "abt%d" % s], ["mrow%d" % s])
        dma(SP, mod_D[0:1, n * 512:(n + 1) * 512], mrow[s][0:1, :], ["mrow%d" % s], ["mod_D"], semkey="st_mod")
        for c4 in range(4):
            mm(psf(6 + s)[:, c4:c4 + 1], mrow[s][0:1, c4 * 128:(c4 + 1) * 128], one_f, True, True,
               ["mrow%d" % s, "one_f"], [psk[6 + s]])
        cp(DVE, modT[:, n * 4:(n + 1) * 4], psf(6 + s)[:, 0:4], [psk[6 + s]], ["modT%d" % (n // 4)])

    for n in range(8):
        ada_tile(n)
    xt = [A.f(2048) for _ in range(2)]
    junk = [A.h(2048) for _ in range(2)]
    xs1 = [A.h(2048) for _ in range(2)]
    stat = [A.f(4) for _ in range(2)]
    xT = [A.h(16 * 512).rearrange("p (k t) -> p k t", k=16) for _ in range(2)]
    stt(DVE, geff1T, modT[:, 16:32], 1.0, g1T_s, ALU.add, ALU.mult, ["modT1", "g1T_s"], ["geff1T"])
    for tc in range(32):
        tg, ti = tc // 4, tc % 4
        s = tc % 2
        if tc % 2 == 0:
            ada_tile(8 + tc // 2)
        dma(SP, xt[s], xh[tc * 128:(tc + 1) * 128, :], [], ["xt%d" % s])
        norm_mod_T(xt[s], "xt%d" % s, geff1T, modT[:, 0:16], ["geff1T", "modT0"], junk[s], xs1[s], stat[s],
                   xT[tg % 2][:, :, ti * 128:(ti + 1) * 128], "xT%d" % (tg % 2), (2 * s, 2 * s + 1), "1%d" % s)
        if ti == 3:
            dma(POOL, xnT_D[:, :, tg * 512:(tg + 1) * 512].rearrange("k p t -> p k t"), xT[tg % 2],
                ["xT%d" % (tg % 2)], ["xnT_D"], semkey="st_xnT")
    P.barrier()

    A = Arena()
    DIL = (1, 4, 16)
    LB = (16, 4, 1)
    WK = [(1 + LB[g]) * 128 for g in range(3)]
    wqkv = A.h(9 * 16 * 128).rearrange("p (c k m) -> p c k m", c=9, k=16)
    qT = [A.h(2048) for _ in range(3)]
    kT = [A.h(DIL[g] * WK[g]) for g in range(3)]
    vT = [A.h(DIL[g] * WK[g]) for g in range(3)]
    vtok = [A.h(DIL[g] * WK[g]).rearrange("p (b d) -> p b d", d=128) for g in range(3)]
    xTs = [A.h(16 * 512).rearrange("p (k t) -> p k t", k=16) for _ in range(2)]
    cs_t = [A.f(512) for _ in range(2)]
    sn_t = [A.f(512) for _ in range(2)]
    qf = [A.h(512) for _ in range(2)]
    t1 = [A.f(512) for _ in range(2)]
    t2 = [A.f(512) for _ in range(2)]
    numden = A.f(4096)
    pexp = [A.h(256) for _ in range(2)]
    pm = [A.h(256) for _ in range(2)]
    rden = A.f(2048)
    aout = A.h(2048)
    pcount = [0]
    pend2 = []
    for j in range(4):
        for typ in range(3):
            for g in range(3):
                col = typ * 1536 + (g * 4 + j) * 128
                dma(POOL, wqkv[:, typ * 3 + g, :, :], w_in[:, col:col + 128].rearrange("(k p) m -> p k m", p=128),
                    [], ["wqkv%d" % (typ * 3 + g)])
        def ld_tg(tgi):
            s = tgi % 2
            dma(SP, xTs[s], xnT_D[:, :, tgi * 512:(tgi + 1) * 512].rearrange("k p t -> p k t"), ["xnT_D"], ["xTs%d" % s])
            dma(SP, cs_t[s], cosT[:, tgi * 512:(tgi + 1) * 512], [], ["cs%d" % s])
            dma(SP, sn_t[s], sinT[:, tgi * 512:(tgi + 1) * 512], [], ["sn%d" % s])

        for tgi in range(8):
            halo = tgi < 4
            s = tgi % 2
            if not (tgi == 0 and j > 0):
                ld_tg(tgi)
            t0 = (tgi % 4) * 512
            for typ in range(3):
                for g in range(3):
                    dil, wk, lb = DIL[g], WK[g], LB[g]
                    if halo:
                        if typ == 0:
                            continue
                        if g < 2 and tgi != 3:
                            continue
                    pc = pcount[0]
                    pcount[0] += 1
                    ba = pc % 2
                    for k in range(16):
                        mm(psf(ba), wqkv[:, typ * 3 + g, k, :], xTs[s][:, k, :], k == 0, k == 15,
                           ["wqkv%d" % (typ * 3 + g), "xTs%d" % s], [psk[ba]])
                    buf = (qT, kT, vT)[typ][g]
                    bkey = ("q%d", "k%d", "v%d")[typ] % g
                    if typ == 0:
                        dst = buf.rearrange("p (r w) -> p r w", r=dil)[:, :, t0 // dil:t0 // dil + 512 // dil]
                        srcsel = (0, 512)
                    elif not halo:
                        dst = buf.rearrange("p (r w) -> p r w", r=dil)[:, :, 128 + t0 // dil:128 + t0 // dil + 512 // dil]
                        srcsel = (0, 512)
                    else:
                        if g == 2:
                            dst = buf.rearrange("p (r w) -> p r w", r=dil)[:, :, t0 // 16:t0 // 16 + 32]
                            srcsel = (0, 512)
                        elif g == 1:
                            dst = buf.rearrange("p (r w) -> p r w", r=dil)[:, :, 0:128]
                            srcsel = (0, 512)
                        else:
                            dst = buf.rearrange("p (r w) -> p r w", r=1)[:, :, 0:128]
                            srcsel = (384, 512)
                    a0, a1 = srcsel

                    def sview(ap2d):
                        return ap2d[:, a0:a1].rearrange("p (l r) -> p r l", r=dil)

                    if pend2:
                        pend2.pop()()
                    if typ == 2:
                        cp(ACT, dst, sview(psf(ba)), [psk[ba]], [bkey])
                    else:
                        bb = 2 + pc % 2
                        fs = pc % 2
                        cp(ACT, qf[fs], psf(ba), [psk[ba]], ["qf%d" % fs])

                        sv1_, sv2_ = sview(t1[fs]), sview(t2[fs])

                        def post(bb=bb, fs=fs, s=s, dst=dst, sv1_=sv1_, sv2_=sv2_, bkey=bkey):
                            mm(psf(bb), rot_b, qf[fs], True, True, ["rot_b", "qf%d" % fs], [psk[bb]])
                            tt(DVE, t1[fs], qf[fs], cs_t[s], ALU.mult, ["qf%d" % fs, "cs%d" % s], ["t1%d" % fs])
                            tt(DVE, t2[fs], psf(bb), sn_t[s], ALU.mult, [psk[bb], "sn%d" % s], ["t2%d" % fs])
                            tt(DVE, dst, sv1_, sv2_, ALU.add, ["t1%d" % fs, "t2%d" % fs], [bkey])

                        pend2.append(post)
        if pend2:
            pend2.pop()()
        for g in range(3):
            nb = DIL[g] * (1 + LB[g])
            for b0 in range(0, nb, 8):
                nbb = min(8, nb - b0)
                for b in range(b0, b0 + nbb):
                    tr(psh(4)[:, (b - b0) * 128:(b - b0 + 1) * 128], vT[g][:, b * 128:(b + 1) * 128], id_b,
                       ["v%d" % g, "id_b"], [psk[4]])
                cp(ACT, vtok[g][:, b0:b0 + nbb, :], psh(4)[:, 0:nbb * 128].rearrange("p (b d) -> p b d", d=128),
                   [psk[4]], ["vtok%d" % g])
        ac = 0
        for g in range(3):
            dil, lb = DIL[g], LB[g]
            for r in range(dil):
                for n in range(lb):
                    s2 = ac % 2
                    ac += 1
                    bc, bd = 4 + s2, 6 + s2
                    qblk = qT[g][:, (r * lb + n) * 128:(r * lb + n + 1) * 128]
                    kb0 = r * (1 + lb) + n
                    for hh in range(2):
                        mm(psf(bc)[:, hh * 128:(hh + 1) * 128], kT[g][:, (kb0 + hh) * 128:(kb0 + hh + 1) * 128], qblk,
                           True, True, ["k%d" % g, "q%d" % g], [psk[bc]])
                    act(pexp[s2], psf(bc)[:, 0:256], AF.Exp, [psk[bc]], ["pexp%d" % s2], scale=float(128 ** -0.5))
                    msk = mf_b if n == 0 else mp_b
                    tt(DVE, pm[s2], pexp[s2], msk, ALU.mult, ["pexp%d" % s2, "mp_b", "mf_b"], ["pm%d" % s2])
                    for hh in range(2):
                        mm(psf(bd)[:, 0:128], vtok[g][:, kb0 + hh, :], pm[s2][:, hh * 128:(hh + 1) * 128],
                           hh == 0, hh == 1, ["vtok%d" % g, "pm%d" % s2], [psk[bd]])
                    for hh in range(2):
                        mm(psf(bd)[:, 128:256], ones_b, pm[s2][:, hh * 128:(hh + 1) * 128],
                           hh == 0, hh == 1, ["ones_b", "pm%d" % s2], [psk[bd]])
                    ov = raw(numden, n * 128 * dil + r, [[2048, 2], [dil, 128]])
                    iv = psf(bd)[:, 0:256].rearrange("p (a b) -> p a b", a=2)
                    if g == 0:
                        cp(DVE, ov, iv, [psk[bd]], ["numden"])
                    else:
                        tt(DVE, ov, ov, iv, ALU.add, [psk[bd], "numden"], ["numden"])
        if j < 3:
            ld_tg(0)
        recip(rden, numden[:, 2048:4096], ["numden"], ["rden"])
        tt(DVE, aout, numden[:, 0:2048], rden, ALU.mult, ["numden", "rden"], ["aout"])
        dma(POOL, attnT_D[j], aout, ["aout"], ["attnT_D"], semkey="st_attn")
    P.barrier()

    A = Arena()
    poolT = A.h(4 * 2048).rearrange("p (g t) -> p g t", g=4)
    off_after_pool = A.off
    wp = A.h(4 * 16 * 128).rearrange("p (g k m) -> p g k m", g=4, k=16)
    mixb = A.h(4 * 128).rearrange("p (g m) -> p g m", g=4)
    pT = A.f(4 * 2064).rearrange("p (g t) -> p g t", g=4)
    sA = A.f(2064)
    sB = A.f(2064)
    dT = A.h(2048)
    dfix = A.f(16)
    xTp = [A.h(16 * 512).rearrange("p (k t) -> p k t", k=16) for _ in range(2)]
    for g in range(4):
        dma(POOL, wp[:, g, :, :], w_in[:, 4608 + g * 128:4608 + (g + 1) * 128].rearrange("(k p) m -> p k m", p=128),
            [], ["wp"], semkey="wp")
        dma(POOL, mixb[:, g, :], pmix[g * 128:(g + 1) * 128, :], [], ["mixb"], semkey="mixb")
    for tgi in range(3, 8):
        s = tgi % 2
        dma(SP, xTp[s], xnT_D[:, :, tgi * 512:(tgi + 1) * 512].rearrange("k p t -> p k t"), ["xnT_D"], ["xTp%d" % s])
        for g in range(4):
            ba = g % 2
            for k in range(16):
                mm(psf(ba), wp[:, g, k, :], xTp[s][:, k, :], k == 0, k == 15, ["wp", "xTp%d" % s], [psk[ba]])
            if tgi == 3:
                amul(pT[:, g, 0:16], psf(ba)[:, 496:512], flag_s, [psk[ba], "flag"], ["pT%d" % g])
            else:
                t0 = (tgi - 4) * 512
                cp(ACT, pT[:, g, 16 + t0:16 + t0 + 512], psf(ba), [psk[ba]], ["pT%d" % g])
    for g in range(4):
        w = (2, 4, 8, 16)[g]
        cur = pT[:, g, :]
        ckey = "pT%d" % g
        sh, lo = 1, 1
        bufs = [sA, sB]
        bi = 0
        while sh < w:
            nxt = bufs[bi]
            nkey = "sbuf%d" % bi
            tt(DVE if bi == 0 else POOL, nxt[:, lo:2064], cur[:, lo:2064], cur[:, lo - sh:2064 - sh], ALU.add,
               [ckey], [nkey])
            cur, ckey = nxt, nkey
            bi ^= 1
            sh *= 2
            lo += sh
        stt(DVE, dT, cur[:, 16:2064], 1.0 / w, pT[:, g, 16:2064], ALU.mult, ALU.subtract, [ckey, "pT%d" % g], ["dT"])
        tt(DVE, dfix, cur[:, 16:32], pcorr_s[:, g * 16:(g + 1) * 16], ALU.mult, [ckey, "pcorr"], ["dfix"])
        tt(DVE, dT[:, 0:16], dfix, pT[:, g, 16:32], ALU.subtract, ["dfix", "pT%d" % g], ["dT"])
        for nt in range(4):
            ba = 2 + nt % 2
            mm(psf(ba), mixb[:, g, :], dT[:, nt * 512:(nt + 1) * 512], True, True, ["mixb", "dT"], [psk[ba]])
            amul(poolT[:, g, nt * 512:(nt + 1) * 512], psf(ba), pscale_s[:, g:g + 1], [psk[ba], "pscale"], ["poolT"])

    P.barrier()
    A.off = off_after_pool
    xTm = A.h(16 * 2048).rearrange("p (k t) -> p k t", k=16)
    atT = A.h(4 * 2048).rearrange("p (k t) -> p k t", k=4)
    wg = [A.h(2 * 16 * 256).rearrange("p (a k m) -> p a k m", a=2, k=16) for _ in range(2)]
    wbr = [A.h(2 * 4 * 256).rearrange("p (a k m) -> p a k m", a=2, k=4) for _ in range(2)]
    sga = [A.f(512) for _ in range(2)]
    sgb = [A.f(512) for _ in range(2)]
    m1 = [A.f(512) for _ in range(2)]
    m2 = [A.f(512) for _ in range(2)]
    mgT = [A.h(512) for _ in range(2)]
    for tgo in range(4):
        dma(SP, xTm[:, :, tgo * 512:(tgo + 1) * 512], xnT_D[:, :, (4 + tgo) * 512:(5 + tgo) * 512].rearrange("k p t -> p k t"),
            ["xnT_D"], ["xTm"], semkey="xTm")
    dma(SP, atT, attnT_D.rearrange("k p t -> p k t"), ["attnT_D"], ["atT"])
    it = 0
    def ld_wgrp(jg):
        ws = jg % 2
        c0 = jg * 256
        dma(POOL, wg[ws][:, 0, :, :], w_in[:, 5120 + c0:5120 + c0 + 256].rearrange("(k p) m -> p k m", p=128),
            [], ["wg%d" % ws], semkey="wg%d" % ws)
        dma(POOL, wg[ws][:, 1, :, :], w_in[:, 7168 + c0:7168 + c0 + 256].rearrange("(k p) m -> p k m", p=128),
            [], ["wg%d" % ws], semkey="wg%d" % ws)
        dma(POOL, wbr[ws][:, 0, :, :], wab[:, c0:c0 + 256].rearrange("(k p) m -> p k m", p=128),
            [], ["wbr%d" % ws], semkey="wbr%d" % ws)
        dma(POOL, wbr[ws][:, 1, :, :], wpb[:, c0:c0 + 256].rearrange("(k p) m -> p k m", p=128),
            [], ["wbr%d" % ws], semkey="wbr%d" % ws)

    ld_wgrp(0)
    for jg in range(8):
        ws = jg % 2
        if jg + 1 < 8:
            ld_wgrp(jg + 1)
        for j4 in range(2):
            jc = jg * 2 + j4
            csl = slice(j4 * 128, (j4 + 1) * 128)
            for tgo in range(4):
                ps_ = it % 2
                it += 1
                bs = 4 * ps_
                tsl = slice(tgo * 512, (tgo + 1) * 512)
                for a in range(2):
                    for k in range(16):
                        mm(psf(bs + a), wg[ws][:, a, k, csl], xTm[:, k, tsl], k == 0, k == 15,
                           ["wg%d" % ws, "xTm"], [psk[bs + a]])
                for k in range(4):
                    mm(psf(bs + 2), wbr[ws][:, 0, k, csl], atT[:, k, tsl], k == 0, k == 3, ["wbr%d" % ws, "atT"], [psk[bs + 2]])
                for k in range(4):
                    mm(psf(bs + 3), wbr[ws][:, 1, k, csl], poolT[:, k, tsl], k == 0, k == 3,
                       ["wbr%d" % ws, "poolT"], [psk[bs + 3]])
                act(sga[ps_], psf(bs + 0), AF.Sigmoid, [psk[bs + 0]], ["sga%d" % ps_])
                act(sgb[ps_], psf(bs + 1), AF.Sigmoid, [psk[bs + 1]], ["sgb%d" % ps_])
                tt(DVE, m1[ps_], sga[ps_], psf(bs + 2), ALU.mult, ["sga%d" % ps_, psk[bs + 2]], ["m1%d" % ps_])
                tt(DVE, m2[ps_], sgb[ps_], psf(bs + 3), ALU.mult, ["sgb%d" % ps_, psk[bs + 3]], ["m2%d" % ps_])
                tt(DVE, mgT[ps_], m1[ps_], m2[ps_], ALU.add, ["m1%d" % ps_, "m2%d" % ps_], ["mgT%d" % ps_])
                dma(SP, mergedT_D[jc, :, tsl], mgT[ps_], ["mgT%d" % ps_], ["mergedT_D"], semkey="st_mg%d" % ps_)
    P.barrier()

    A = Arena()
    wo = A.h(16 * 2048).rearrange("p (k m) -> p k m", k=16)
    g1g = A.f(2048)
    mT = [A.h(16 * 128).rearrange("p (k t) -> p k t", k=16) for _ in range(2)]
    xo = [A.f(2048) for _ in range(2)]
    h1 = [A.f(2048) for _ in range(2)]
    junk2 = [A.h(2048) for _ in range(2)]
    xs2 = [A.h(2048) for _ in range(2)]
    stat2 = [A.f(4) for _ in range(2)]
    xT2 = [A.h(16 * 512).rearrange("p (k t) -> p k t", k=16) for _ in range(2)]
    for dn in range(4):
        dma(POOL, wo[:, :, dn * 512:(dn + 1) * 512], w_out[:, dn * 512:(dn + 1) * 512].rearrange("(k p) m -> p k m", p=128),
            [], ["wo%d" % dn])
    bcast_row(g1g, mod_D[0:1, 4096:6144], "g1g", rd=["mod_D"])
    stt(DVE, geff2T, modT[:, 64:80], 1.0, g2T_s, ALU.add, ALU.mult, ["modT4", "g2T_s"], ["geff2T"])
    def b2_A(tc, part):
        s = tc % 2
        if part == 0:
            dma(SP, mT[s], mergedT_D[:, :, tc * 128:(tc + 1) * 128].rearrange("k p t -> p k t"), ["mergedT_D"], ["mT%d" % s])
            dma(SP, xo[s], xh[2048 + tc * 128:2048 + (tc + 1) * 128, :], [], ["xo%d" % s])
        for dn in ((0, 1) if part == 0 else (2, 3)):
            for k in range(16):
                mm(psf(dn), mT[s][:, k, :], wo[:, k, dn * 512:(dn + 1) * 512], k == 0, k == 15, ["mT%d" % s, "wo%d" % dn], [psk[dn]])
            tt(DVE, h1[s][:, dn * 512:(dn + 1) * 512], psf(dn), g1g[:, dn * 512:(dn + 1) * 512], ALU.mult,
               [psk[dn], "g1g"], ["h1p%d" % s])
        if part == 1:
            tt(POOL, h1[s], h1[s], xo[s], ALU.add, ["h1p%d" % s, "xo%d" % s], ["h1%d" % s])
            dma(POOL, h1_D[tc * 128:(tc + 1) * 128, :], h1[s], ["h1%d" % s], ["h1_D"], semkey="st_h1")

    def b2_B(tc, stage):
        s = tc % 2
        tg, ti = tc // 4, tc % 4
        norm_mod_T(h1[s], "h1%d" % s, geff2T, modT[:, 48:64], ["geff2T", "modT3"], junk2[s], xs2[s], stat2[s],
                   xT2[tg % 2][:, :, ti * 128:(ti + 1) * 128], "xT2%d" % (tg % 2), (4 + 2 * s, 5 + 2 * s), "2%d" % s, stage=stage)
        if stage == 2 and ti == 3:
            dma(POOL, xn2T_D[:, :, tg * 512:(tg + 1) * 512].rearrange("k p t -> p k t"), xT2[tg % 2],
                ["xT2%d" % (tg % 2)], ["xn2T_D"], semkey="st_xn2T")

    b2_A(0, 0)
    b2_A(0, 1)
    for tc in range(16):
        if tc + 1 < 16:
            b2_A(tc + 1, 0)
        b2_B(tc, 1)
        if tc + 1 < 16:
            b2_A(tc + 1, 1)
        b2_B(tc, 2)
    P.barrier()

    A = Arena()
    wqb = A.h(16 * 2048).rearrange("p (k m) -> p k m", k=16)
    kTf = A.f(2048).rearrange("p (c n) -> p c n", c=16)
    xq = [A.h(16 * 128).rearrange("p (k t) -> p k t", k=16) for _ in range(2)]
    qTf = [A.f(16 * 128).rearrange("p (c t) -> p c t", c=16) for _ in range(2)]
    ssb = [A.f(2048) for _ in range(2)]
    spl = [[A.h(2048) for _ in range(3)] for _ in range(2)]
    sres = [A.f(2048) for _ in range(2)]
    for cg in range(4):
        dma(POOL, wqb[:, :, cg * 512:(cg + 1) * 512], wq[:, cg * 512:(cg + 1) * 512].rearrange("(k p) m -> p k m", p=128),
            [], ["wqb%d" % cg])
    dma(SP, kTf, keysT.rearrange("p (c n) -> p c n", c=16), [], ["kTf"])
    for tc in range(16):
        s = tc % 2
        dma(SP, xq[s], xn2T_D[:, :, tc * 128:(tc + 1) * 128].rearrange("k p t -> p k t"), ["xn2T_D"], ["xq%d" % s])
        for cc in range(16):
            ba = cc % 2
            for k in range(16):
                mm(psf(ba)[:, 0:128], wqb[:, k, cc * 128:(cc + 1) * 128], xq[s][:, k, :], k == 0, k == 15,
                   ["wqb%d" % (cc // 4), "xq%d" % s], [psk[ba]])
            cp(ACT if cc % 2 == 0 else DVE, qTf[s][:, cc, :], psf(ba)[:, 0:128], [psk[ba]], ["qTf%d" % s])
        for cc in range(16):
            bk = 4 + cc // 4
            mm(psf(bk)[:, (cc % 4) * 128:(cc % 4 + 1) * 128], qTf[s][:, cc, :], kTf[:, cc, :], True, True,
               ["qTf%d" % s, "kTf"], [psk[bk]])
        for q4 in range(4):
            cp(ACT if q4 % 2 == 0 else DVE, ssb[s][:, q4 * 512:(q4 + 1) * 512], psf(4 + q4), [psk[4 + q4]], ["ssb%d" % s])
        dma(POOL, s_D[tc].rearrange("c t n -> t c n"), ssb[s].rearrange("p (c n) -> p c n", c=16), ["ssb%d" % s], ["s_D"], semkey="st_s")
        cp(ACT, spl[s][0], ssb[s], ["ssb%d" % s], ["spl%d_0" % s])
        tt(DVE, sres[s], ssb[s], spl[s][0], ALU.subtract, ["ssb%d" % s, "spl%d_0" % s], ["sres%d" % s])
        cp(ACT, spl[s][1], sres[s], ["sres%d" % s], ["spl%d_1" % s])
        tt(DVE, sres[s], sres[s], spl[s][1], ALU.subtract, ["sres%d" % s, "spl%d_1" % s], ["sres%d" % s])
        cp(ACT, spl[s][2], sres[s], ["sres%d" % s], ["spl%d_2" % s])
        for pc in range(3):
            dma(POOL, s3_D[tc, pc].rearrange("c t n -> t c n"), spl[s][pc].rearrange("p (c n) -> p c n", c=16),
                ["spl%d_%d" % (s, pc)], ["s3_D"], semkey="st_s3")
    P.barrier()

    A = Arena()
    s_sb = A.f(2048)
    stmp = A.f(2048)
    a16 = A.f(256)
    cand = A.f(2048)
    ctmp = A.f(2048)
    c16 = A.f(128)
    csub = A.f(128)
    e16 = A.f(128)
    zz = A.f(8)
    lnz = A.f(8)
    nbias = A.f(8)
    tokv = A.f(384)
    slotv = [A.f(384) for _ in range(2)]
    s1h = [A.h(4096) for _ in range(2)]
    s2h = [A.h(4096) for _ in range(2)]
    NG = 3
    ohb = [A.h(512) for _ in range(NG)]
    uub = [A.f(512) for _ in range(NG)]
    mkb = [A.h(512) for _ in range(NG)]
    eeb = [A.h(512) for _ in range(NG)]
    RRb = [A.h(512) for _ in range(NG)]
    Gt = [A.h(16384) for _ in range(2)]
    gcount = 0
    bcount = 0

    def v3(ap2d):
        return ap2d.rearrange("p (t n) -> p t n", t=8)

    def topk_gen(tc):
        S2 = s_sb
        ks = "s_sb"
        dma(SP, S2.rearrange("p (c n) -> p c n", c=16), s_D[tc].rearrange("c t n -> t c n"), ["s_D"], [ks])
        yield
        for cc in range(16):
            vmax(a16[:, cc * 16:cc * 16 + 8], S2[:, cc * 128:(cc + 1) * 128], [ks], ["a16"])
            yield
            vmr(stmp[:, cc * 128:(cc + 1) * 128], a16[:, cc * 16:cc * 16 + 8], S2[:, cc * 128:(cc + 1) * 128], [ks, "a16"], ["stmp"])
            yield
            vmax(a16[:, cc * 16 + 8:cc * 16 + 16], stmp[:, cc * 128:(cc + 1) * 128], ["stmp"], ["a16"])
            yield
        tt(DVE, cand.rearrange("p (h i j) -> p h i j", h=8, i=16),
           raw(a16, 0, [[32, 8], [1, 16], [0, 16]]), raw(a16, 16, [[32, 8], [0, 16], [1, 16]]), ALU.add, ["a16"], ["cand"])
        yield
        for h in range(8):
            vmax(c16[:, h * 16:h * 16 + 8], cand[:, h * 256:(h + 1) * 256], ["cand"], ["c16"])
            yield
            vmr(ctmp[:, h * 256:(h + 1) * 256], c16[:, h * 16:h * 16 + 8], cand[:, h * 256:(h + 1) * 256], ["cand", "c16"], ["ctmp"])
            yield
            vmax(c16[:, h * 16 + 8:h * 16 + 16], ctmp[:, h * 256:(h + 1) * 256], ["ctmp"], ["c16"])
            yield
        tt(DVE, csub.rearrange("p (h i) -> p h i", h=8), c16.rearrange("p (h i) -> p h i", h=8),
           raw(c16, 0, [[16, 8], [0, 16]]), ALU.subtract, ["c16"], ["csub"])
        yield
        act(e16, csub, AF.Exp, ["csub"], ["e16"])
        yield
        P.op(DVE, lambda e: e.reduce_sum(out=zz, in_=e16.rearrange("p (h i) -> p h i", h=8), axis=AX.X), reads=["e16"], writes=["zz"])
        yield
        act(lnz, zz, AF.Ln, ["zz"], ["lnz"])
        yield
        stt(DVE, nbias, raw(c16, 0, [[16, 8]]), -1.0, lnz, ALU.mult, ALU.subtract, ["c16", "lnz"], ["nbias"])
        yield
        a1v = raw(a16, 0, [[32, 8], [1, 16]])
        cp(DVE, tokv[:, 0:128].rearrange("p (h i) -> p h i", h=8), a1v, ["a16"], ["tokv"])
        yield
        cp(DVE, tokv[:, 128:256].rearrange("p (h i) -> p h i", h=8), raw(c16, 15, [[16, 8], [0, 16]]), ["c16"], ["tokv"])
        yield
        tt(DVE, tokv[:, 256:384].rearrange("p (h i) -> p h i", h=8), a1v, raw(nbias, 0, [[1, 8], [0, 16]]), ALU.add,
           ["a16", "nbias"], ["tokv"])
        yield
        sv = slotv[tc % 2]
        ksv = "slotv%d" % (tc % 2)
        for a in range(3):
            tr(psf(7)[:, a * 128:(a + 1) * 128], tokv[:, a * 128:(a + 1) * 128], id_f, ["tokv", "id_f"], [psk[7]])
            yield
        cp(ACT, sv, psf(7)[:, 0:384], [psk[7]], [ksv])
        yield

    def run_gen(g, k=None):
        n_ = 0
        while g is not None and (k is None or n_ < k):
            try:
                next(g)
            except StopIteration:
                return None
            n_ += 1
        return g

    run_gen(topk_gen(0))
    for tc in range(16):
        sv = slotv[tc % 2]
        ksv = "slotv%d" % (tc % 2)
        nxt = topk_gen(tc + 1) if tc + 1 < 16 else None
        gs = tc % 2
        Gt3 = Gt[gs].rearrange("p (c t) -> p c t", c=128)
        def stA(q, tc=tc):
            tb, sg = q // 8, q % 8
            rs = tb % 2
            if sg == 0:
                for p_ in range(2):
                    for pc in range(3):
                        srcb = s3_D[tc, pc, p_]
                        src = AP(srcb.tensor, srcb.offset + (tb * 32) * 128, [[2 * 16384, 8], [1, 4096]])
                        dma(SP, (s1h, s2h)[p_][rs][pc * 8:(pc + 1) * 8, :], src, ["s3_D"], ["sh%d_%d" % (p_, rs)],
                            semkey="sh%d_%d" % (p_, rs))
            st = q % 2
            b1, b2 = 2 * st, 2 * st + 1
            mm(psf(b1), rep_b, s1h[rs][0:24, sg * 512:(sg + 1) * 512], True, True, ["rep_b", "sh0_%d" % rs], [psk[b1]])
            mm(psf(b2), rep_b, s2h[rs][0:24, sg * 512:(sg + 1) * 512], True, True, ["rep_b", "sh1_%d" % rs], [psk[b2]])

        def stB(q, sv=sv, ksv=ksv):
            gi, st, tl = q % NG, q % 2, q * 4
            b1, b2 = 2 * st, 2 * st + 1
            oh3 = ohb[gi].rearrange("p (t n) -> p t n", t=4)
            mk3 = mkb[gi].rearrange("p (t n) -> p t n", t=4)
            ee3 = eeb[gi].rearrange("p (t n) -> p t n", t=4)
            tt(DVE, oh3, psf(b1).rearrange("p (t n) -> p t n", t=4), raw(sv, tl, [[1, 4], [0, 128]]), ALU.is_equal,
               [psk[b1], ksv], ["oh%d" % gi])
            uu3 = uub[gi].rearrange("p (t n) -> p t n", t=4)
            tt(DVE, uu3, psf(b2).rearrange("p (t n) -> p t n", t=4), raw(sv, tl, [[1, 4], [0, 128]]), ALU.add,
               [psk[b2], ksv], ["uu%d" % gi])
            tt(DVE, mk3, uu3, raw(sv, 128 + tl, [[1, 4], [0, 128]]), ALU.is_ge, ["uu%d" % gi, ksv], ["mk%d" % gi])
            for t4 in range(4):
                col = tl + t4
                act(ee3[:, t4, :], psf(b2)[:, t4 * 128:(t4 + 1) * 128], AF.Exp, [psk[b2], ksv], ["ee%d" % gi],
                    bias=sv[:, 256 + col:256 + col + 1], scale=1.0)

        def stC(q):
            gi = q % NG
            tt(POOL, RRb[gi], mkb[gi], eeb[gi], ALU.mult, ["mk%d" % gi, "ee%d" % gi], ["RR%d" % gi])

        def stD(q):
            gi, bo = q % NG, 4 + q % 3
            oh3 = ohb[gi].rearrange("p (t n) -> p t n", t=4)
            RR3 = RRb[gi].rearrange("p (t n) -> p t n", t=4)
            for t4 in range(4):
                mm(psf(bo).rearrange("p (c t) -> p t c", t=4)[:, t4, :], oh3[:, t4, :], RR3[:, t4, :], True, True,
                   ["oh%d" % gi, "RR%d" % gi], [psk[bo]])

        def stE(q, Gt3=Gt3, gs=gs):
            bo, tl = 4 + q % 3, q * 4
            cp(ACT, Gt3[:, :, tl:tl + 4], psf(bo).rearrange("p (c t) -> p c t", t=4), [psk[bo]], ["Gt%d" % gs])

        NQ = 32
        for n in range(NQ + 3):
            if n < NQ:
                stA(n)
            if 0 <= n - 1 < NQ:
                stB(n - 1)
            if 0 <= n - 2 < NQ:
                stC(n - 2)
                stD(n - 2)
            if 0 <= n - 3 < NQ:
                stE(n - 3)
            nxt = run_gen(nxt, 4)
        run_gen(nxt)
        dma(POOL, G_D[tc], Gt[gs], ["Gt%d" % gs], ["G_D"], semkey="st_G")
    P.barrier()

    A = Arena()
    x2r = A.f(8192)
    x2h = x2r[:, :].bitcast(BF16).rearrange("p (k t) -> p k t", k=16)
    h1q = [x2r[:, i * 2048:(i + 1) * 2048] for i in range(4)]
    oacc = A.f(8 * 2048).rearrange("p (c m) -> p c m", c=8)
    wdr = [A.f(4096) for _ in range(2)]
    wur = [A.f(4096) for _ in range(2)]
    wd = [w_[:, :].bitcast(BF16).rearrange("p (k e) -> p k e", k=16) for w_ in wdr]
    wu = [w_[:, :].bitcast(BF16).rearrange("p (c m) -> p c m", c=4) for w_ in wur]
    GlT = [A.h(2048).rearrange("p (a c t) -> p a c t", a=4, c=4) for _ in range(3)]
    gel = [A.h(2048).rearrange("p (c t) -> p c t", c=4) for _ in range(2)]
    Am = [A.h(2048).rearrange("p (a c t) -> p a c t", a=4, c=4) for _ in range(2)]
    stat5 = [A.f(4) for _ in range(2)]
    h1l = [wdr[0][:, 0:2048], wdr[0][:, 2048:4096]]
    yyl = [wdr[1][:, 0:2048], wdr[1][:, 2048:4096]]
    oo = [wur[0][:, 0:2048], wur[0][:, 2048:4096]]
    g2g = wur[1][:, 0:2048]
    gfb = wur[1][:, 2048:4096]
    pend5 = []
    it5c = [0]

    def p5_front(H, et, tgl, i5):
        ws, gsl, s2 = et % 2, i5 % 3, i5 % 2
        for a in range(4):
            tcg = H * 8 + tgl * 4 + a
            dma(SP, GlT[gsl][:, a, :, :], G_D[tcg, :, et * 512:(et + 1) * 512].rearrange("p (c t) -> p c t", c=4),
                ["G_D"], ["GlT%d" % gsl], semkey="GlT%d" % gsl)
        for c4 in range(4):
            for k in range(16):
                mm(psf(c4), wd[ws][:, k, c4 * 128:(c4 + 1) * 128], x2h[:, k, tgl * 512:(tgl + 1) * 512], k == 0, k == 15,
                   ["x2h", "wd%d" % ws], [psk[c4]])
            act(gel[s2][:, c4, :], psf(c4), AF.Gelu, [psk[c4]], ["gel%d" % s2])
        for a in range(4):
            tt(DVE, Am[s2][:, a, :, :], gel[s2][:, :, a * 128:(a + 1) * 128], GlT[gsl][:, a, :, :], ALU.mult,
               ["gel%d" % s2, "GlT%d" % gsl], ["Am%d" % s2])

    def p5_back(H, et, tgl, i5):
        ws, s2 = et % 2, i5 % 2
        for a in range(4):
            tcl = tgl * 4 + a
            for dn in range(4):
                for c4 in range(4):
                    mm(psf(4 + dn), Am[s2][:, a, c4, :], wu[ws][:, c4, dn * 512:(dn + 1) * 512], c4 == 0, c4 == 3,
                       ["Am%d" % s2, "wu%d" % ws], [psk[4 + dn]])
            for dn in range(4):
                ov = oacc[:, tcl, dn * 512:(dn + 1) * 512]
                if et == 0:
                    cp(DVE, ov, psf(4 + dn), [psk[4 + dn]], ["oacc%d" % tcl])
                else:
                    tt(DVE, ov, ov, psf(4 + dn), ALU.add, [psk[4 + dn], "oacc%d" % tcl], ["oacc%d" % tcl])

    def p5_iter(H, et, tgl):
        i5 = it5c[0]
        it5c[0] += 1
        p5_front(H, et, tgl, i5)
        if pend5:
            p5_back(*pend5.pop())
        pend5.append((H, et, tgl, i5))

    for H in range(2):
        dma(SP, x2h, xn2T_D[:, :, H * 1024:(H + 1) * 1024].rearrange("k p t -> p k t"), ["xn2T_D"], ["x2h"])
        for et in range(32):
            ws = et % 2
            dma(POOL, wd[ws], wdT[:, et * 512:(et + 1) * 512].rearrange("(k p) e -> p k e", p=128), [], ["wd%d" % ws])
            dma(POOL, wu[ws], wup[et * 512:(et + 1) * 512, :].rearrange("(c p) m -> p c m", p=128), [], ["wu%d" % ws])
            for tgl in range(2):
                p5_iter(H, et, tgl)
        if pend5:
            p5_back(*pend5.pop())
        P.barrier()
        dma(SP, g2g, mod_D[0:1, 10240:12288].partition_broadcast(128), ["mod_D"], ["e_g2g"])
        dma(SP, gfb, gf[0:1, :].partition_broadcast(128), [], ["e_gfb"])
        junk5b = [gel[0].rearrange("p c t -> p (c t)"), gel[1].rearrange("p c t -> p (c t)")]
        def ld_h1(tcl, H=H):
            tcg = H * 8 + tcl
            dma(SP, h1q[tcl % 4], h1_D[tcg * 128:(tcg + 1) * 128, :], ["h1_D"], ["e_h1%d" % (tcl % 4)])

        for tcl in range(3):
            ld_h1(tcl)

        def ep_X(tcl, H=H):
            s = tcl % 2
            yv = yyl[s]
            if tcl + 3 < 8:
                ld_h1(tcl + 3)
            tt(DVE, yv, oacc[:, tcl, :], g2g, ALU.mult, ["oacc%d" % tcl, "e_g2g"], ["e_y%d" % s])
            tt(POOL, yv, yv, h1q[tcl % 4], ALU.add, ["e_y%d" % s, "e_h1%d" % (tcl % 4)], ["e_y%d" % s])
            ss, rt, rstd = stat5[s][:, 0:1], stat5[s][:, 1:2], stat5[s][:, 2:3]
            act(junk5b[s], yv, AF.Square, ["e_y%d" % s], ["e_j%d" % s, "e_ss%d" % s], accum=ss)
            act(rt, ss, AF.Sqrt, ["e_ss%d" % s], ["e_rt%d" % s], bias=1e-6, scale=1.0 / 2048)
            recip(rstd, rt, ["e_rt%d" % s], ["e_rs%d" % s])

        def ep_Y(tcl, H=H):
            s = tcl % 2
            yv = yyl[s]
            tcg = H * 8 + tcl
            rstd = stat5[s][:, 2:3]
            amul(yv, yv, rstd, ["e_y%d" % s, "e_rs%d" % s], ["e_y%d" % s])
            tt(DVE, oo[s], yv, gfb, ALU.mult, ["e_y%d" % s, "e_gfb"], ["e_o%d" % s])
            dma(SP, out[tcg * 128:(tcg + 1) * 128, :], oo[s], ["e_o%d" % s], ["out"], semkey="st_out%d" % s)

        ep_X(0)
        for tcl in range(8):
            if tcl + 1 < 8:
                ep_X(tcl + 1)
            ep_Y(tcl)
        P.barrier()

    P.emit()
    es.close()
    return nc


_CACHE = {}


def _host_consts():
    if "c" in _CACHE:
        return _CACHE["c"]
    ident = np.eye(128, dtype=np.float32)
    R = np.zeros((128, 128), np.float32)
    for d in range(64):
        R[d, d + 64] = -1.0
        R[d + 64, d] = 1.0
    rotT = np.ascontiguousarray(R.T)
    k = np.arange(128)[:, None]
    q = np.arange(128)[None, :]
    mprev = (k >= q).astype(np.float32)
    mcur = (k <= q).astype(np.float32)
    half = 64
    inv = np.power(np.float32(10000.0), -np.arange(half, dtype=np.float32) / np.float32(half)).astype(np.float32)
    _CACHE["c"] = dict(ident=ident, rotT=rotT, mprev=mprev, mcur=mcur, inv=inv)
    return _CACHE["c"]


def kernel(x, c, ada_w, ada_b, norm1_g, w_in, pool_mix, pool_scale, w_attn_branch, w_pool_branch, w_out,
           norm2_g, peer_wq, peer_keys, peer_down, peer_up, final_g, _debug=()):
    f32 = np.float32
    x = np.asarray(x, f32)
    hc = _host_consts()
    xs = x[0]
    shared = dict(
        c_in=np.ascontiguousarray(np.asarray(c, f32).reshape(16, 128).T),
        ada_w=np.ascontiguousarray(np.asarray(ada_w, f32)[0]),
        ada_b=np.ascontiguousarray(np.asarray(ada_b, f32).reshape(1, 12288)),
        g1=np.asarray(norm1_g, f32).reshape(1, 2048),
        g2=np.asarray(norm2_g, f32).reshape(1, 2048),
        gf=np.asarray(final_g, f32).reshape(1, 2048),
        g1T=np.ascontiguousarray(np.asarray(norm1_g, f32).reshape(16, 128).T),
        g2T=np.ascontiguousarray(np.asarray(norm2_g, f32).reshape(16, 128).T),
        w_in=np.ascontiguousarray(np.asarray(w_in, f32)[0]),
        pmix=np.ascontiguousarray(np.asarray(pool_mix, f32).reshape(512, 128)),
        pscale=np.ascontiguousarray(np.asarray(pool_scale, f32).reshape(4, 128).T),
        wab=np.ascontiguousarray(np.asarray(w_attn_branch, f32)[0]),
        wpb=np.ascontiguousarray(np.asarray(w_pool_branch, f32)[0]),
        w_out=np.ascontiguousarray(np.asarray(w_out, f32)[0]),
        wq=np.ascontiguousarray(np.asarray(peer_wq, f32)[0]),
        keysT=np.ascontiguousarray(np.asarray(peer_keys, f32)[0].reshape(16, 128, 128).transpose(2, 0, 1).reshape(128, 2048)),
        wdT=np.ascontiguousarray(np.asarray(peer_down, f32)[0].reshape(128, 128, 2048).transpose(2, 1, 0).reshape(2048, 16384)),
        wup=np.ascontiguousarray(np.asarray(peer_up, f32)[0].reshape(128, 128, 2048).transpose(1, 0, 2).reshape(16384, 2048)),
        ident=hc["ident"], rotT=hc["rotT"], repm=np.ascontiguousarray(np.tile(np.kron(np.eye(8, dtype=np.float32), np.ones((1, 16), np.float32)), (3, 1))), mprev=hc["mprev"], mcur=hc["mcur"],
    )
    in_maps = []
    for core in range(NCORES):
        base = core * 2048
        xh = np.zeros((4096, 2048), f32)
        if core > 0:
            xh[:2048] = xs[base - 2048:base]
        xh[2048:] = xs[base:base + 2048]
        pos = (np.arange(4096) + (base - 2048)).astype(f32)
        ang = (pos[:, None] * hc["inv"][None, :]).astype(f32)
        cosT = np.ascontiguousarray(np.concatenate([np.cos(ang), np.cos(ang)], axis=1).T.astype(f32))
        sinT = np.ascontiguousarray(np.concatenate([np.sin(ang), np.sin(ang)], axis=1).T.astype(f32))
        pc = np.zeros((4, 16), f32)
        for g, w in enumerate((2, 4, 8, 16)):
            for t in range(16):
                pc[g, t] = 1.0 / (min(t + 1, w) if core == 0 else w)
        m = dict(shared)
        m.update(
            xh=xh, cosT=cosT, sinT=sinT,
            mfirst=(hc["mprev"] if core > 0 else np.zeros((128, 128), f32)),
            flag=np.full((128, 1), 1.0 if core > 0 else 0.0, f32),
            pcorr=np.ascontiguousarray(np.broadcast_to(pc.reshape(1, 64), (128, 64))),
        )
        in_maps.append(m)
    key = ("nc", tuple(_debug))
    if key not in _CACHE:
        _CACHE[key] = build(debug=tuple(_debug))
    nc = _CACHE[key]
    res = run_bass_kernel_spmd(nc, in_maps, core_ids=list(range(NCORES)))
    outs = [np.asarray(r["out"], f32) for r in res.results]
    full = np.concatenate(outs, axis=0).reshape(1, 16384, 2048)
    if _debug:
        DBG["res"] = res.results
    return full
```

```python
import numpy as np
from contextlib import ExitStack
import concourse.bass as bass
import concourse.mybir as mybir
from concourse.ap import AP
from concourse.bass_utils import run_bass_kernel_spmd

F32 = mybir.dt.float32
BF16 = mybir.dt.bfloat16
AF = mybir.ActivationFunctionType
ALU = mybir.AluOpType
AX = mybir.AxisListType
PE, ACT, DVE, POOL, SP = "pe", "act", "dve", "pool", "sp"
NCORES = 8


class _Op:
    __slots__ = ("eng", "fn", "deps", "dma", "semkey", "has_dep", "ticket", "idx")


def _is_ps(k):
    return isinstance(k, str) and k.startswith("ps")


class Prog:
    def __init__(self, nc):
        self.nc = nc
        self.ops = []
        self.last_w = {}
        self.readers = {}
        self.pending = {}
        self.last_on_eng = {}
        self.last_dma = {}

    def barrier(self):
        deps = set(self.last_on_eng.values()) | set(self.last_dma.values())
        for e in (PE, ACT, DVE, POOL, SP):
            self.pending[e] = set(deps)

    def op(self, eng, fn, reads=(), writes=(), dma=False, semkey=None):
        o = _Op()
        o.eng, o.fn, o.dma, o.has_dep, o.ticket = eng, fn, dma, False, None
        o.idx = len(self.ops)
        reads = list(reads)
        writes = list(writes)
        pr = [k for k in reads if _is_ps(k)]
        if pr:
            reads = [k for k in reads if not _is_ps(k)]
            writes = writes + pr
        deps = set()
        for k in reads:
            w = self.last_w.get(k)
            if w is not None:
                deps.add(w)
        for k in writes:
            w = self.last_w.get(k)
            if w is not None:
                deps.add(w)
            for r in self.readers.get(k, ()):
                deps.add(r)
        pend = self.pending.pop(eng, None)
        if pend:
            deps |= pend
        deps.discard(o.idx)
        o.deps = deps
        if dma:
            sk = semkey if semkey is not None else (writes[0] if writes else reads[0])
            o.semkey = (eng, sk)
            self.last_dma[o.semkey] = o.idx
        else:
            o.semkey = None
            self.last_on_eng[eng] = o.idx
        for k in reads:
            self.readers.setdefault(k, []).append(o.idx)
        for k in writes:
            self.last_w[k] = o.idx
            self.readers[k] = []
        self.ops.append(o)
        return o

    def emit(self):
        nc = self.nc
        ops = self.ops
        for o in ops:
            nd = set()
            for d in o.deps:
                p = ops[d]
                if (not p.dma) and (not o.dma) and p.eng == o.eng and o.eng == PE:
                    continue
                nd.add(d)
            o.deps = nd
            for d in nd:
                ops[d].has_dep = True
        with ExitStack() as es:
            engsem = {e: es.enter_context(nc.semaphore("s_" + e)) for e in (PE, ACT, DVE, POOL, SP)}
            dmasem = {}
            for o in ops:
                if o.dma and o.semkey not in dmasem:
                    dmasem[o.semkey] = es.enter_context(nc.semaphore("d%d" % len(dmasem)))
            cnt = {e: 0 for e in engsem}
            dcnt = {k: 0 for k in dmasem}
            for o in ops:
                if o.dma:
                    dcnt[o.semkey] += 16
                    o.ticket = dcnt[o.semkey]
                elif o.has_dep:
                    cnt[o.eng] += 1
                    o.ticket = cnt[o.eng]
            running = {k: 0 for k in dmasem}
            waits = []
            for o in ops:
                ws = {}
                for d in o.deps:
                    p = ops[d]
                    if p.dma:
                        key, val = ("d", p.semkey), running[p.semkey]
                    else:
                        key, val = ("e", p.eng), p.ticket
                    if ws.get(key, 0) < val:
                        ws[key] = val
                waits.append(ws)
                if o.dma:
                    running[o.semkey] = o.ticket
            final = dict(running)
            self.n_sems = len(engsem) + len(dmasem)
            engmap = {PE: "tensor", ACT: "scalar", DVE: "vector", POOL: "gpsimd", SP: "sync"}
            with nc.Block() as block:
                for e in (SP, POOL, ACT, DVE, PE):
                    myops = [o for o in ops if o.eng == e]

                    def body(eng, e=e, myops=myops):
                        waited = {}
                        for o in myops:
                            for key, val in waits[o.idx].items():
                                if waited.get(key, 0) >= val:
                                    continue
                                waited[key] = val
                                sem = dmasem[key[1]] if key[0] == "d" else engsem[key[1]]
                                eng.wait_ge(sem, val)
                            ins = o.fn(eng)
                            if o.dma:
                                ins.then_inc(dmasem[o.semkey], 16)
                            elif o.has_dep:
                                ins.then_inc(engsem[e], 1)
                        if e == SP:
                            for k, v in final.items():
                                if v > 0 and waited.get(("d", k), 0) < v:
                                    eng.wait_ge(dmasem[k], v)
                            for e2 in (PE, ACT, DVE, POOL):
                                if cnt[e2] > 0:
                                    eng.wait_ge(engsem[e2], cnt[e2])

                    getattr(block, engmap[e])(body)


def raw(ap2d, col_off, dims):
    return AP(ap2d.tensor, ap2d.offset + col_off, [list(ap2d.ap[0])] + [list(d) for d in dims])


DBG = {}


def build(debug=()):
    nc = bass.Bass("TRN2", target_bir_lowering=False)
    P = Prog(nc)

    def din(name, shape, dt=F32):
        return nc.dram_tensor(name, list(shape), dt, kind="ExternalInput").ap()

    def dscr(name, shape, dt):
        kind = "ExternalOutput" if name in debug else "Internal"
        return nc.dram_tensor(name, list(shape), dt, kind=kind).ap()

    xh = din("xh", [4096, 2048])
    cosT = din("cosT", [128, 4096])
    sinT = din("sinT", [128, 4096])
    c_in = din("c_in", [128, 16])
    ada_w = din("ada_w", [2048, 12288])
    ada_b = din("ada_b", [1, 12288])
    g1 = din("g1", [1, 2048])
    g2 = din("g2", [1, 2048])
    gf = din("gf", [1, 2048])
    g1T = din("g1T", [128, 16])
    g2T = din("g2T", [128, 16])
    w_in = din("w_in", [2048, 9216])
    pmix = din("pmix", [512, 128])
    pscale = din("pscale", [128, 4])
    wab = din("wab", [512, 2048])
    wpb = din("wpb", [512, 2048])
    w_out = din("w_out", [2048, 2048])
    wq = din("wq", [2048, 2048])
    keysT = din("keysT", [128, 2048])
    wdT = din("wdT", [2048, 16384])
    wup = din("wup", [16384, 2048])
    ident = din("ident", [128, 128])
    rotT = din("rotT", [128, 128])
    mprev = din("mprev", [128, 128])
    mcur = din("mcur", [128, 128])
    mfirst = din("mfirst", [128, 128])
    flag = din("flag", [128, 1])
    pcorr = din("pcorr", [128, 64])
    repm = din("repm", [24, 128])
    out = nc.dram_tensor("out", [2048, 2048], F32, kind="ExternalOutput").ap()

    mod_D = dscr("mod_D", [1, 12288], F32)
    xnT_D = dscr("xnT_D", [16, 128, 4096], BF16)
    attnT_D = dscr("attnT_D", [4, 128, 2048], BF16)
    mergedT_D = dscr("mergedT_D", [16, 128, 2048], BF16)
    h1_D = dscr("h1_D", [2048, 2048], F32)
    xn2T_D = dscr("xn2T_D", [16, 128, 2048], BF16)
    s_D = dscr("s_D", [16, 16, 128, 128], F32)
    G_D = dscr("G_D", [16, 128, 16384], BF16)
    s3_D = dscr("s3_D", [16, 3, 16, 128, 128], BF16)

    es = ExitStack()
    SLABF = 49152
    slab = es.enter_context(nc.sbuf_tensor("slab", [128, SLABF], F32))
    cst = es.enter_context(nc.sbuf_tensor("cst", [128, 1280], F32))
    psb = [es.enter_context(nc.psum_tensor("psb%d" % i, [128, 512], F32)) for i in range(8)]
    psk = ["ps%d" % i for i in range(8)]

    def psf(i):
        return psb[i][:]

    def psh(i):
        return psb[i][:].bitcast(BF16)

    class Arena:
        def __init__(self):
            self.off = 0

        def f(self, n):
            v = slab[:, self.off:self.off + n]
            self.off += n
            assert self.off <= SLABF, self.off
            return v

        def h(self, n):
            assert n % 2 == 0
            v = slab[:, self.off:self.off + n // 2].bitcast(BF16)
            self.off += n // 2
            assert self.off <= SLABF, self.off
            return v

    def mm(o, lhsT, rhs, start, stop, r, w):
        P.op(PE, lambda e: e.matmul(o, lhsT=lhsT, rhs=rhs, start=start, stop=stop), reads=r, writes=w)

    def tr(o, in_, idn, r, w):
        P.op(PE, lambda e: e.transpose(out=o, in_=in_, identity=idn), reads=r, writes=w)

    def act(o, in_, func, r, w, bias=None, scale=None, accum=None):
        kw = {}
        if bias is not None:
            kw["bias"] = bias
        if scale is not None:
            kw["scale"] = scale
        if accum is not None:
            kw["accum_out"] = accum
        P.op(ACT, lambda e: e.activation(out=o, in_=in_, func=func, **kw), reads=r, writes=w)

    def amul(o, in_, m, r, w):
        P.op(ACT, lambda e: e.mul(out=o, in_=in_, mul=m), reads=r, writes=w)

    def tt(eng, o, a, b, op, r, w):
        P.op(eng, lambda e: e.tensor_tensor(out=o, in0=a, in1=b, op=op), reads=r, writes=w)

    def stt(eng, o, a, s, b, op0, op1, r, w):
        P.op(eng, lambda e: e.scalar_tensor_tensor(out=o, in0=a, scalar=s, in1=b, op0=op0, op1=op1), reads=r, writes=w)

    def tsc(eng, o, a, s1, op0, r, w):
        P.op(eng, lambda e: e.tensor_scalar(out=o, in0=a, scalar1=s1, scalar2=None, op0=op0), reads=r, writes=w)

    def cp(eng, o, in_, r, w):
        if eng == ACT:
            P.op(ACT, lambda e: e.activation(out=o, in_=in_, func=AF.Copy), reads=r, writes=w)
        else:
            P.op(eng, lambda e: e.tensor_copy(out=o, in_=in_), reads=r, writes=w)

    def dma(eng, o, in_, r, w, semkey=None):
        P.op(eng, lambda e: e.dma_start(out=o, in_=in_), reads=r, writes=w, dma=True, semkey=semkey)

    def recip(o, in_, r, w):
        P.op(DVE, lambda e: e.reciprocal(out=o, in_=in_), reads=r, writes=w)

    def vmax(o, in_, r, w):
        P.op(DVE, lambda e: e.max(out=o, in_=in_), reads=r, writes=w)

    def vmr(o, rep, vals, r, w):
        P.op(DVE, lambda e: e.match_replace(out=o, in_to_replace=rep, in_values=vals, imm_value=-1e30), reads=r, writes=w)

    def memset(eng, o, val, w):
        P.op(eng, lambda e: e.memset(o, val), reads=[], writes=w)

    id_f = cst[:, 0:128]
    rot_f = cst[:, 128:256]
    id_b = cst[:, 256:320].bitcast(BF16)
    mp_b = cst[:, 320:448].bitcast(BF16)
    mf_b = cst[:, 448:576].bitcast(BF16)
    ones_b = cst[:, 576:640].bitcast(BF16)
    flag_s = cst[:, 640:641]
    pcorr_s = cst[:, 704:768]
    pscale_s = cst[:, 768:772]
    csil = cst[:, 800:816]
    craw = cst[:, 816:832]
    rep_b = cst[0:24, 832:896].bitcast(BF16)
    dma(SP, id_f, ident, [], ["id_f"])
    dma(SP, rot_f, rotT, [], ["rot_f"])
    dma(POOL, id_b, ident, [], ["id_b"])
    rot_b = cst[:, 1130:1194].bitcast(BF16)
    dma(POOL, rot_b, rotT, [], ["rot_b"])
    dma(POOL, mp_b[:, 0:128], mprev, [], ["mp_b"], semkey="mp_b")
    dma(POOL, mp_b[:, 128:256], mcur, [], ["mp_b"], semkey="mp_b")
    dma(POOL, mf_b[:, 0:128], mfirst, [], ["mf_b"], semkey="mf_b")
    dma(POOL, mf_b[:, 128:256], mcur, [], ["mf_b"], semkey="mf_b")
    dma(SP, flag_s, flag, [], ["flag"])
    dma(SP, pcorr_s, pcorr, [], ["pcorr"])
    dma(SP, pscale_s, pscale, [], ["pscale"])
    dma(SP, craw, c_in, [], ["craw"])
    modT = cst[:, 960:1056]
    one_f = cst[0:1, 1056:1057]
    g1T_s = cst[:, 1060:1076]
    g2T_s = cst[:, 1076:1092]
    geff1T = cst[:, 1092:1108]
    geff2T = cst[:, 1108:1124]
    memset(DVE, one_f, 1.0, ["one_f"])
    dma(SP, g1T_s, g1T, [], ["g1T_s"])
    dma(SP, g2T_s, g2T, [], ["g2T_s"])
    dma(POOL, rep_b, repm, [], ["rep_b"])
    memset(DVE, ones_b, 1.0, ["ones_b"])
    act(csil, craw, AF.Silu, ["craw"], ["csil"])

    def bcast_row(dst, src_row, key, rd=()):
        dma(SP, dst, src_row.partition_broadcast(128), list(rd), [key])

    def norm_mod_T(xt, kx, geffT, shT, kaff, junk, xs, stat, dstv, kdst, banks, tag, stage=0):
        ss, rt, rstd = stat[:, 0:1], stat[:, 1:2], stat[:, 2:3]
        b0, b1 = banks
        if stage in (0, 1):
            act(junk, xt, AF.Square, [kx], [tag + "junk", tag + "ss"], accum=ss)
            act(rt, ss, AF.Sqrt, [tag + "ss"], [tag + "rt"], bias=1e-6, scale=1.0 / 2048)
            recip(rstd, rt, [tag + "rt"], [tag + "rstd"])
            tsc(DVE, xs, xt, rstd, ALU.mult, [kx, tag + "rstd"], [tag + "xs"])
            for k in range(16):
                b = b0 if k < 8 else b1
                tr(psh(b)[:, (k % 8) * 128:(k % 8 + 1) * 128], xs[:, k * 128:(k + 1) * 128], id_b,
                   [tag + "xs", "id_b"], [psk[b]])
        if stage == 1:
            return
        for k in range(16):
            b = b0 if k < 8 else b1
            src = psh(b)[:, (k % 8) * 128:(k % 8 + 1) * 128]
            if k < 8:
                act(dstv[:, k, :], src, AF.Identity, [psk[b]] + kaff, [kdst], bias=shT[:, k:k + 1], scale=geffT[:, k:k + 1])
            else:
                P.op(DVE, lambda e, o=dstv[:, k, :], a=src, s1_=geffT[:, k:k + 1], s2_=shT[:, k:k + 1]:
                     e.tensor_scalar(out=o, in0=a, scalar1=s1_, scalar2=s2_, op0=ALU.mult, op1=ALU.add),
                     reads=[psk[b]] + kaff, writes=[kdst])

    A = Arena()
    NAW = 4
    aw = [A.h(16 * 512).rearrange("p (k m) -> p k m", k=16) for _ in range(NAW)]
    abt = [A.f(512) for _ in range(2)]
    mrow = [A.f(512) for _ in range(2)]
    csil_b = A.h(16)
    cp(DVE, csil_b, csil, ["csil"], ["csil_b"])

    def ada_tile(n):
        s = n % 2
        sw = n % NAW
        dma(POOL, aw[sw], ada_w[:, n * 512:(n + 1) * 512].rearrange("(k p) m -> p k m", p=128), [], ["aw%d" % sw])
        dma(SP, abt[s][0:1, :], ada_b[0:1, n * 512:(n + 1) * 512], [], ["abt%d" % s])
        for k in range(16):
            mm(psf(6 + s)[0:1, :], csil_b[:, k:k + 1], aw[sw][:, k, :], k == 0, k == 15, ["csil_b", "aw%d" % sw], [psk[6 + s]])
        tt(DVE, mrow[s][0:1, :], psf(6 + s)[0:1, :], abt[s][0:1, :], ALU.add, [psk[6 + s], "abt%d" % s], ["mrow%d" % s])
        dma(SP, mod_D[0:1, n * 512:(n + 1) * 512], mrow[s][0:1, :], ["mrow%d" % s], ["mod_D"], semkey="st_mod")
        for c4 in range(4):
            mm(psf(6 + s)[:, c4:c4 + 1], mrow[s][0:1, c4 * 128:(c4 + 1) * 128], one_f, True, True,
               ["mrow%d" % s, "one_f"], [psk[6 + s]])
        cp(DVE, modT[:, n * 4:(n + 1) * 4], psf(6 + s)[:, 0:4], [psk[6 + s]], ["modT%d" % (n // 4)])

    for n in range(8):
        ada_tile(n)
    xt = [A.f(2048) for _ in range(2)]
    junk = [A.h(2048) for _ in range(2)]
    xs1 = [A.h(2048) for _ in range(2)]
    stat = [A.f(4) for _ in range(2)]
    xT = [A.h(16 * 512).rearrange("p (k t) -> p k t", k=16) for _ in range(2)]
    stt(DVE, geff1T, modT[:, 16:32], 1.0, g1T_s, ALU.add, ALU.mult, ["modT1", "g1T_s"], ["geff1T"])
    for tc in range(32):
        tg, ti = tc // 4, tc % 4
        s = tc % 2
        if tc % 2 == 0:
            ada_tile(8 + tc // 2)
        dma(SP, xt[s], xh[tc * 128:(tc + 1) * 128, :], [], ["xt%d" % s])
        norm_mod_T(xt[s], "xt%d" % s, geff1T, modT[:, 0:16], ["geff1T", "modT0"], junk[s], xs1[s], stat[s],
                   xT[tg % 2][:, :, ti * 128:(ti + 1) * 128], "xT%d" % (tg % 2), (2 * s, 2 * s + 1), "1%d" % s)
        if ti == 3:
            dma(POOL, xnT_D[:, :, tg * 512:(tg + 1) * 512].rearrange("k p t -> p k t"), xT[tg % 2],
                ["xT%d" % (tg % 2)], ["xnT_D"], semkey="st_xnT")
    P.barrier()

    A = Arena()
    DIL = (1, 4, 16)
    LB = (16, 4, 1)
    WK = [(1 + LB[g]) * 128 for g in range(3)]
    wqkv = A.h(9 * 16 * 128).rearrange("p (c k m) -> p c k m", c=9, k=16)
    qT = [A.h(2048) for _ in range(3)]
    kT = [A.h(DIL[g] * WK[g]) for g in range(3)]
    vT = [A.h(DIL[g] * WK[g]) for g in range(3)]
    vtok = [A.h(DIL[g] * WK[g]).rearrange("p (b d) -> p b d", d=128) for g in range(3)]
    xTs = [A.h(16 * 512).rearrange("p (k t) -> p k t", k=16) for _ in range(2)]
    cs_t = [A.f(512) for _ in range(2)]
    sn_t = [A.f(512) for _ in range(2)]
    qf = [A.h(512) for _ in range(2)]
    t1 = [A.f(512) for _ in range(2)]
    t2 = [A.f(512) for _ in range(2)]
    numden = A.f(4096)
    pexp = [A.h(256) for _ in range(2)]
    pm = [A.h(256) for _ in range(2)]
    rden = A.f(2048)
    aout = A.h(2048)
    pcount = [0]
    pend2 = []
    for j in range(4):
        for typ in range(3):
            for g in range(3):
                col = typ * 1536 + (g * 4 + j) * 128
                dma(POOL, wqkv[:, typ * 3 + g, :, :], w_in[:, col:col + 128].rearrange("(k p) m -> p k m", p=128),
                    [], ["wqkv%d" % (typ * 3 + g)])
        def ld_tg(tgi):
            s = tgi % 2
            dma(SP, xTs[s], xnT_D[:, :, tgi * 512:(tgi + 1) * 512].rearrange("k p t -> p k t"), ["xnT_D"], ["xTs%d" % s])
            dma(SP, cs_t[s], cosT[:, tgi * 512:(tgi + 1) * 512], [], ["cs%d" % s])
            dma(SP, sn_t[s], sinT[:, tgi * 512:(tgi + 1) * 512], [], ["sn%d" % s])

        for tgi in range(8):
            halo = tgi < 4
            s = tgi % 2
            if not (tgi == 0 and j > 0):
                ld_tg(tgi)
            t0 = (tgi % 4) * 512
            for typ in range(3):
                for g in range(3):
                    dil, wk, lb = DIL[g], WK[g], LB[g]
                    if halo:
                        if typ == 0:
                            continue
                        if g < 2 and tgi != 3:
                            continue
                    pc = pcount[0]
                    pcount[0] += 1
                    ba = pc % 2
                    for k in range(16):
                        mm(psf(ba), wqkv[:, typ * 3 + g, k, :], xTs[s][:, k, :], k == 0, k == 15,
                           ["wqkv%d" % (typ * 3 + g), "xTs%d" % s], [psk[ba]])
                    buf = (qT, kT, vT)[typ][g]
                    bkey = ("q%d", "k%d", "v%d")[typ] % g
                    if typ == 0:
                        dst = buf.rearrange("p (r w) -> p r w", r=dil)[:, :, t0 // dil:t0 // dil + 512 // dil]
                        srcsel = (0, 512)
                    elif not halo:
                        dst = buf.rearrange("p (r w) -> p r w", r=dil)[:, :, 128 + t0 // dil:128 + t0 // dil + 512 // dil]
                        srcsel = (0, 512)
                    else:
                        if g == 2:
                            dst = buf.rearrange("p (r w) -> p r w", r=dil)[:, :, t0 // 16:t0 // 16 + 32]
                            srcsel = (0, 512)
                        elif g == 1:
                            dst = buf.rearrange("p (r w) -> p r w", r=dil)[:, :, 0:128]
                            srcsel = (0, 512)
                        else:
                            dst = buf.rearrange("p (r w) -> p r w", r=1)[:, :, 0:128]
                            srcsel = (384, 512)
                    a0, a1 = srcsel

                    def sview(ap2d):
                        return ap2d[:, a0:a1].rearrange("p (l r) -> p r l", r=dil)

                    if pend2:
                        pend2.pop()()
                    if typ == 2:
                        cp(ACT, dst, sview(psf(ba)), [psk[ba]], [bkey])
                    else:
                        bb = 2 + pc % 2
                        fs = pc % 2
                        cp(ACT, qf[fs], psf(ba), [psk[ba]], ["qf%d" % fs])

                        sv1_, sv2_ = sview(t1[fs]), sview(t2[fs])

                        def post(bb=bb, fs=fs, s=s, dst=dst, sv1_=sv1_, sv2_=sv2_, bkey=bkey):
                            mm(psf(bb), rot_b, qf[fs], True, True, ["rot_b", "qf%d" % fs], [psk[bb]])
                            tt(DVE, t1[fs], qf[fs], cs_t[s], ALU.mult, ["qf%d" % fs, "cs%d" % s], ["t1%d" % fs])
                            tt(DVE, t2[fs], psf(bb), sn_t[s], ALU.mult, [psk[bb], "sn%d" % s], ["t2%d" % fs])
                            tt(POOL, dst, sv1_, sv2_, ALU.add, ["t1%d" % fs, "t2%d" % fs], [bkey])

                        pend2.append(post)
        if pend2:
            pend2.pop()()
        for g in range(3):
            nb = DIL[g] * (1 + LB[g])
            for b0 in range(0, nb, 8):
                nbb = min(8, nb - b0)
                for b in range(b0, b0 + nbb):
                    tr(psh(4)[:, (b - b0) * 128:(b - b0 + 1) * 128], vT[g][:, b * 128:(b + 1) * 128], id_b,
                       ["v%d" % g, "id_b"], [psk[4]])
                cp(ACT, vtok[g][:, b0:b0 + nbb, :], psh(4)[:, 0:nbb * 128].rearrange("p (b d) -> p b d", d=128),
                   [psk[4]], ["vtok%d" % g])
        ac = 0
        for g in range(3):
            dil, lb = DIL[g], LB[g]
            for r in range(dil):
                for n in range(lb):
                    s2 = ac % 2
                    ac += 1
                    bc, bd = 4 + s2, 6 + s2
                    qblk = qT[g][:, (r * lb + n) * 128:(r * lb + n + 1) * 128]
                    kb0 = r * (1 + lb) + n
                    for hh in range(2):
                        mm(psf(bc)[:, hh * 128:(hh + 1) * 128], kT[g][:, (kb0 + hh) * 128:(kb0 + hh + 1) * 128], qblk,
                           True, True, ["k%d" % g, "q%d" % g], [psk[bc]])
                    act(pexp[s2], psf(bc)[:, 0:256], AF.Exp, [psk[bc]], ["pexp%d" % s2], scale=float(128 ** -0.5))
                    msk = mf_b if n == 0 else mp_b
                    tt(DVE, pm[s2], pexp[s2], msk, ALU.mult, ["pexp%d" % s2, "mp_b", "mf_b"], ["pm%d" % s2])
                    for hh in range(2):
                        mm(psf(bd)[:, 0:128], vtok[g][:, kb0 + hh, :], pm[s2][:, hh * 128:(hh + 1) * 128],
                           hh == 0, hh == 1, ["vtok%d" % g, "pm%d" % s2], [psk[bd]])
                    for hh in range(2):
                        mm(psf(bd)[:, 128:256], ones_b, pm[s2][:, hh * 128:(hh + 1) * 128],
                           hh == 0, hh == 1, ["ones_b", "pm%d" % s2], [psk[bd]])
                    ov = raw(numden, n * 128 * dil + r, [[2048, 2], [dil, 128]])
                    iv = psf(bd)[:, 0:256].rearrange("p (a b) -> p a b", a=2)
                    if g == 0:
                        cp(DVE, ov, iv, [psk[bd]], ["numden"])
                    else:
                        tt(DVE, ov, ov, iv, ALU.add, [psk[bd], "numden"], ["numden"])
        if j < 3:
            ld_tg(0)
        recip(rden, numden[:, 2048:4096], ["numden"], ["rden"])
        tt(DVE, aout, numden[:, 0:2048], rden, ALU.mult, ["numden", "rden"], ["aout"])
        dma(POOL, attnT_D[j], aout, ["aout"], ["attnT_D"], semkey="st_attn")
    P.barrier()

    A = Arena()
    poolT = A.h(4 * 2048).rearrange("p (g t) -> p g t", g=4)
    off_after_pool = A.off
    wp = A.h(4 * 16 * 128).rearrange("p (g k m) -> p g k m", g=4, k=16)
    mixb = A.h(4 * 128).rearrange("p (g m) -> p g m", g=4)
    pT = A.f(4 * 2064).rearrange("p (g t) -> p g t", g=4)
    sA = A.f(2064)
    sB = A.f(2064)
    dT = A.h(2048)
    dfix = A.f(16)
    xTp = [A.h(16 * 512).rearrange("p (k t) -> p k t", k=16) for _ in range(2)]
    for g in range(4):
        dma(POOL, wp[:, g, :, :], w_in[:, 4608 + g * 128:4608 + (g + 1) * 128].rearrange("(k p) m -> p k m", p=128),
            [], ["wp"], semkey="wp")
        dma(POOL, mixb[:, g, :], pmix[g * 128:(g + 1) * 128, :], [], ["mixb"], semkey="mixb")
    for tgi in range(3, 8):
        s = tgi % 2
        dma(SP, xTp[s], xnT_D[:, :, tgi * 512:(tgi + 1) * 512].rearrange("k p t -> p k t"), ["xnT_D"], ["xTp%d" % s])
        for g in range(4):
            ba = g % 2
            for k in range(16):
                mm(psf(ba), wp[:, g, k, :], xTp[s][:, k, :], k == 0, k == 15, ["wp", "xTp%d" % s], [psk[ba]])
            if tgi == 3:
                amul(pT[:, g, 0:16], psf(ba)[:, 496:512], flag_s, [psk[ba], "flag"], ["pT%d" % g])
            else:
                t0 = (tgi - 4) * 512
                cp(ACT, pT[:, g, 16 + t0:16 + t0 + 512], psf(ba), [psk[ba]], ["pT%d" % g])
    for g in range(4):
        w = (2, 4, 8, 16)[g]
        cur = pT[:, g, :]
        ckey = "pT%d" % g
        sh, lo = 1, 1
        bufs = [sA, sB]
        bi = 0
        while sh < w:
            nxt = bufs[bi]
            nkey = "sbuf%d" % bi
            tt(DVE if bi == 0 else POOL, nxt[:, lo:2064], cur[:, lo:2064], cur[:, lo - sh:2064 - sh], ALU.add,
               [ckey], [nkey])
            cur, ckey = nxt, nkey
            bi ^= 1
            sh *= 2
            lo += sh
        stt(DVE, dT, cur[:, 16:2064], 1.0 / w, pT[:, g, 16:2064], ALU.mult, ALU.subtract, [ckey, "pT%d" % g], ["dT"])
        tt(DVE, dfix, cur[:, 16:32], pcorr_s[:, g * 16:(g + 1) * 16], ALU.mult, [ckey, "pcorr"], ["dfix"])
        tt(DVE, dT[:, 0:16], dfix, pT[:, g, 16:32], ALU.subtract, ["dfix", "pT%d" % g], ["dT"])
        for nt in range(4):
            ba = 2 + nt % 2
            mm(psf(ba), mixb[:, g, :], dT[:, nt * 512:(nt + 1) * 512], True, True, ["mixb", "dT"], [psk[ba]])
            amul(poolT[:, g, nt * 512:(nt + 1) * 512], psf(ba), pscale_s[:, g:g + 1], [psk[ba], "pscale"], ["poolT"])

    P.barrier()
    A.off = off_after_pool
    xTm = A.h(16 * 2048).rearrange("p (k t) -> p k t", k=16)
    atT = A.h(4 * 2048).rearrange("p (k t) -> p k t", k=4)
    wg = [A.h(2 * 16 * 256).rearrange("p (a k m) -> p a k m", a=2, k=16) for _ in range(2)]
    wbr = [A.h(2 * 4 * 256).rearrange("p (a k m) -> p a k m", a=2, k=4) for _ in range(2)]
    sga = [A.f(512) for _ in range(2)]
    sgb = [A.f(512) for _ in range(2)]
    m1 = [A.f(512) for _ in range(2)]
    m2 = [A.f(512) for _ in range(2)]
    mgT = [A.h(512) for _ in range(2)]
    for tgo in range(4):
        dma(SP, xTm[:, :, tgo * 512:(tgo + 1) * 512], xnT_D[:, :, (4 + tgo) * 512:(5 + tgo) * 512].rearrange("k p t -> p k t"),
            ["xnT_D"], ["xTm"], semkey="xTm")
    dma(SP, atT, attnT_D.rearrange("k p t -> p k t"), ["attnT_D"], ["atT"])
    it = 0
    def ld_wgrp(jg):
        ws = jg % 2
        c0 = jg * 256
        dma(POOL, wg[ws][:, 0, :, :], w_in[:, 5120 + c0:5120 + c0 + 256].rearrange("(k p) m -> p k m", p=128),
            [], ["wg%d" % ws], semkey="wg%d" % ws)
        dma(POOL, wg[ws][:, 1, :, :], w_in[:, 7168 + c0:7168 + c0 + 256].rearrange("(k p) m -> p k m", p=128),
            [], ["wg%d" % ws], semkey="wg%d" % ws)
        dma(POOL, wbr[ws][:, 0, :, :], wab[:, c0:c0 + 256].rearrange("(k p) m -> p k m", p=128),
            [], ["wbr%d" % ws], semkey="wbr%d" % ws)
        dma(POOL, wbr[ws][:, 1, :, :], wpb[:, c0:c0 + 256].rearrange("(k p) m -> p k m", p=128),
            [], ["wbr%d" % ws], semkey="wbr%d" % ws)

    ld_wgrp(0)
    for jg in range(8):
        ws = jg % 2
        if jg + 1 < 8:
            ld_wgrp(jg + 1)
        for j4 in range(2):
            jc = jg * 2 + j4
            csl = slice(j4 * 128, (j4 + 1) * 128)
            for tgo in range(4):
                ps_ = it % 2
                it += 1
                bs = 4 * ps_
                tsl = slice(tgo * 512, (tgo + 1) * 512)
                for a in range(2):
                    for k in range(16):
                        mm(psf(bs + a), wg[ws][:, a, k, csl], xTm[:, k, tsl], k == 0, k == 15,
                           ["wg%d" % ws, "xTm"], [psk[bs + a]])
                for k in range(4):
                    mm(psf(bs + 2), wbr[ws][:, 0, k, csl], atT[:, k, tsl], k == 0, k == 3, ["wbr%d" % ws, "atT"], [psk[bs + 2]])
                for k in range(4):
                    mm(psf(bs + 3), wbr[ws][:, 1, k, csl], poolT[:, k, tsl], k == 0, k == 3,
                       ["wbr%d" % ws, "poolT"], [psk[bs + 3]])
                act(sga[ps_], psf(bs + 0), AF.Sigmoid, [psk[bs + 0]], ["sga%d" % ps_])
                act(sgb[ps_], psf(bs + 1), AF.Sigmoid, [psk[bs + 1]], ["sgb%d" % ps_])
                tt(DVE, m1[ps_], sga[ps_], psf(bs + 2), ALU.mult, ["sga%d" % ps_, psk[bs + 2]], ["m1%d" % ps_])
                tt(DVE, m2[ps_], sgb[ps_], psf(bs + 3), ALU.mult, ["sgb%d" % ps_, psk[bs + 3]], ["m2%d" % ps_])
                tt(DVE, mgT[ps_], m1[ps_], m2[ps_], ALU.add, ["m1%d" % ps_, "m2%d" % ps_], ["mgT%d" % ps_])
                dma(SP, mergedT_D[jc, :, tsl], mgT[ps_], ["mgT%d" % ps_], ["mergedT_D"], semkey="st_mg%d" % ps_)
    P.barrier()

    A = Arena()
    wo = A.h(16 * 2048).rearrange("p (k m) -> p k m", k=16)
    g1g = A.f(2048)
    mT = [A.h(16 * 128).rearrange("p (k t) -> p k t", k=16) for _ in range(2)]
    xo = [A.f(2048) for _ in range(2)]
    h1 = [A.f(2048) for _ in range(2)]
    junk2 = [A.h(2048) for _ in range(2)]
    xs2 = [A.h(2048) for _ in range(2)]
    stat2 = [A.f(4) for _ in range(2)]
    xT2 = [A.h(16 * 512).rearrange("p (k t) -> p k t", k=16) for _ in range(2)]
    for dn in range(4):
        dma(POOL, wo[:, :, dn * 512:(dn + 1) * 512], w_out[:, dn * 512:(dn + 1) * 512].rearrange("(k p) m -> p k m", p=128),
            [], ["wo%d" % dn])
    bcast_row(g1g, mod_D[0:1, 4096:6144], "g1g", rd=["mod_D"])
    stt(DVE, geff2T, modT[:, 64:80], 1.0, g2T_s, ALU.add, ALU.mult, ["modT4", "g2T_s"], ["geff2T"])
    def b2_A(tc, part):
        s = tc % 2
        if part == 0:
            dma(SP, mT[s], mergedT_D[:, :, tc * 128:(tc + 1) * 128].rearrange("k p t -> p k t"), ["mergedT_D"], ["mT%d" % s])
            dma(SP, xo[s], xh[2048 + tc * 128:2048 + (tc + 1) * 128, :], [], ["xo%d" % s])
        for dn in ((0, 1) if part == 0 else (2, 3)):
            for k in range(16):
                mm(psf(dn), mT[s][:, k, :], wo[:, k, dn * 512:(dn + 1) * 512], k == 0, k == 15, ["mT%d" % s, "wo%d" % dn], [psk[dn]])
            tt(DVE, h1[s][:, dn * 512:(dn + 1) * 512], psf(dn), g1g[:, dn * 512:(dn + 1) * 512], ALU.mult,
               [psk[dn], "g1g"], ["h1p%d" % s])
        if part == 1:
            tt(POOL, h1[s], h1[s], xo[s], ALU.add, ["h1p%d" % s, "xo%d" % s], ["h1%d" % s])
            dma(POOL, h1_D[tc * 128:(tc + 1) * 128, :], h1[s], ["h1%d" % s], ["h1_D"], semkey="st_h1")

    def b2_B(tc, stage):
        s = tc % 2
        tg, ti = tc // 4, tc % 4
        norm_mod_T(h1[s], "h1%d" % s, geff2T, modT[:, 48:64], ["geff2T", "modT3"], junk2[s], xs2[s], stat2[s],
                   xT2[tg % 2][:, :, ti * 128:(ti + 1) * 128], "xT2%d" % (tg % 2), (4 + 2 * s, 5 + 2 * s), "2%d" % s, stage=stage)
        if stage == 2 and ti == 3:
            dma(POOL, xn2T_D[:, :, tg * 512:(tg + 1) * 512].rearrange("k p t -> p k t"), xT2[tg % 2],
                ["xT2%d" % (tg % 2)], ["xn2T_D"], semkey="st_xn2T")

    b2_A(0, 0)
    b2_A(0, 1)
    for tc in range(16):
        if tc + 1 < 16:
            b2_A(tc + 1, 0)
        b2_B(tc, 1)
        if tc + 1 < 16:
            b2_A(tc + 1, 1)
        b2_B(tc, 2)
    P.barrier()

    A = Arena()
    wqb = A.h(16 * 2048).rearrange("p (k m) -> p k m", k=16)
    kTf = A.f(2048).rearrange("p (c n) -> p c n", c=16)
    xq = [A.h(16 * 128).rearrange("p (k t) -> p k t", k=16) for _ in range(2)]
    qTf = [A.f(16 * 128).rearrange("p (c t) -> p c t", c=16) for _ in range(2)]
    ssb = [A.f(2048) for _ in range(2)]
    spl = [[A.h(2048) for _ in range(3)] for _ in range(2)]
    sres = [A.f(2048) for _ in range(2)]
    for cg in range(4):
        dma(POOL, wqb[:, :, cg * 512:(cg + 1) * 512], wq[:, cg * 512:(cg + 1) * 512].rearrange("(k p) m -> p k m", p=128),
            [], ["wqb%d" % cg])
    dma(SP, kTf, keysT.rearrange("p (c n) -> p c n", c=16), [], ["kTf"])
    for tc in range(16):
        s = tc % 2
        dma(SP, xq[s], xn2T_D[:, :, tc * 128:(tc + 1) * 128].rearrange("k p t -> p k t"), ["xn2T_D"], ["xq%d" % s])
        for cc in range(16):
            ba = cc % 2
            for k in range(16):
                mm(psf(ba)[:, 0:128], wqb[:, k, cc * 128:(cc + 1) * 128], xq[s][:, k, :], k == 0, k == 15,
                   ["wqb%d" % (cc // 4), "xq%d" % s], [psk[ba]])
            cp(ACT if cc % 2 == 0 else DVE, qTf[s][:, cc, :], psf(ba)[:, 0:128], [psk[ba]], ["qTf%d" % s])
        for cc in range(16):
            bk = 4 + cc // 4
            mm(psf(bk)[:, (cc % 4) * 128:(cc % 4 + 1) * 128], qTf[s][:, cc, :], kTf[:, cc, :], True, True,
               ["qTf%d" % s, "kTf"], [psk[bk]])
        for q4 in range(4):
            cp(ACT if q4 % 2 == 0 else DVE, ssb[s][:, q4 * 512:(q4 + 1) * 512], psf(4 + q4), [psk[4 + q4]], ["ssb%d" % s])
        dma(POOL, s_D[tc].rearrange("c t n -> t c n"), ssb[s].rearrange("p (c n) -> p c n", c=16), ["ssb%d" % s], ["s_D"], semkey="st_s")
        cp(ACT, spl[s][0], ssb[s], ["ssb%d" % s], ["spl%d_0" % s])
        tt(DVE, sres[s], ssb[s], spl[s][0], ALU.subtract, ["ssb%d" % s, "spl%d_0" % s], ["sres%d" % s])
        cp(ACT, spl[s][1], sres[s], ["sres%d" % s], ["spl%d_1" % s])
        tt(DVE, sres[s], sres[s], spl[s][1], ALU.subtract, ["sres%d" % s, "spl%d_1" % s], ["sres%d" % s])
        cp(ACT, spl[s][2], sres[s], ["sres%d" % s], ["spl%d_2" % s])
        for pc in range(3):
            dma(POOL, s3_D[tc, pc].rearrange("c t n -> t c n"), spl[s][pc].rearrange("p (c n) -> p c n", c=16),
                ["spl%d_%d" % (s, pc)], ["s3_D"], semkey="st_s3")
    P.barrier()

    A = Arena()
    s_sb = A.f(2048)
    stmp = A.f(2048)
    a16 = A.f(256)
    cand = A.f(2048)
    ctmp = A.f(2048)
    c16 = A.f(128)
    csub = A.f(128)
    e16 = A.f(128)
    zz = A.f(8)
    lnz = A.f(8)
    nbias = A.f(8)
    tokv = A.f(384)
    slotv = [A.f(384) for _ in range(2)]
    s1h = [A.h(4096) for _ in range(2)]
    s2h = [A.h(4096) for _ in range(2)]
    NG = 3
    ohb = [A.h(512) for _ in range(NG)]
    uub = [A.f(512) for _ in range(NG)]
    mkb = [A.h(512) for _ in range(NG)]
    eeb = [A.h(512) for _ in range(NG)]
    RRb = [A.h(512) for _ in range(NG)]
    Gt = [A.h(16384) for _ in range(2)]
    gcount = 0
    bcount = 0

    def v3(ap2d):
        return ap2d.rearrange("p (t n) -> p t n", t=8)

    def topk_gen(tc):
        S2 = s_sb
        ks = "s_sb"
        dma(SP, S2.rearrange("p (c n) -> p c n", c=16), s_D[tc].rearrange("c t n -> t c n"), ["s_D"], [ks])
        yield
        for cc in range(16):
            vmax(a16[:, cc * 16:cc * 16 + 8], S2[:, cc * 128:(cc + 1) * 128], [ks], ["a16"])
            yield
            vmr(stmp[:, cc * 128:(cc + 1) * 128], a16[:, cc * 16:cc * 16 + 8], S2[:, cc * 128:(cc + 1) * 128], [ks, "a16"], ["stmp"])
            yield
            vmax(a16[:, cc * 16 + 8:cc * 16 + 16], stmp[:, cc * 128:(cc + 1) * 128], ["stmp"], ["a16"])
            yield
        tt(DVE, cand.rearrange("p (h i j) -> p h i j", h=8, i=16),
           raw(a16, 0, [[32, 8], [1, 16], [0, 16]]), raw(a16, 16, [[32, 8], [0, 16], [1, 16]]), ALU.add, ["a16"], ["cand"])
        yield
        for h in range(8):
            vmax(c16[:, h * 16:h * 16 + 8], cand[:, h * 256:(h + 1) * 256], ["cand"], ["c16"])
            yield
            vmr(ctmp[:, h * 256:(h + 1) * 256], c16[:, h * 16:h * 16 + 8], cand[:, h * 256:(h + 1) * 256], ["cand", "c16"], ["ctmp"])
            yield
            vmax(c16[:, h * 16 + 8:h * 16 + 16], ctmp[:, h * 256:(h + 1) * 256], ["ctmp"], ["c16"])
            yield
        tt(DVE, csub.rearrange("p (h i) -> p h i", h=8), c16.rearrange("p (h i) -> p h i", h=8),
           raw(c16, 0, [[16, 8], [0, 16]]), ALU.subtract, ["c16"], ["csub"])
        yield
        act(e16, csub, AF.Exp, ["csub"], ["e16"])
        yield
        P.op(DVE, lambda e: e.reduce_sum(out=zz, in_=e16.rearrange("p (h i) -> p h i", h=8), axis=AX.X), reads=["e16"], writes=["zz"])
        yield
        act(lnz, zz, AF.Ln, ["zz"], ["lnz"])
        yield
        stt(DVE, nbias, raw(c16, 0, [[16, 8]]), -1.0, lnz, ALU.mult, ALU.subtract, ["c16", "lnz"], ["nbias"])
        yield
        a1v = raw(a16, 0, [[32, 8], [1, 16]])
        cp(DVE, tokv[:, 0:128].rearrange("p (h i) -> p h i", h=8), a1v, ["a16"], ["tokv"])
        yield
        cp(DVE, tokv[:, 128:256].rearrange("p (h i) -> p h i", h=8), raw(c16, 15, [[16, 8], [0, 16]]), ["c16"], ["tokv"])
        yield
        tt(DVE, tokv[:, 256:384].rearrange("p (h i) -> p h i", h=8), a1v, raw(nbias, 0, [[1, 8], [0, 16]]), ALU.add,
           ["a16", "nbias"], ["tokv"])
        yield
        sv = slotv[tc % 2]
        ksv = "slotv%d" % (tc % 2)
        for a in range(3):
            tr(psf(7)[:, a * 128:(a + 1) * 128], tokv[:, a * 128:(a + 1) * 128], id_f, ["tokv", "id_f"], [psk[7]])
            yield
        cp(ACT, sv, psf(7)[:, 0:384], [psk[7]], [ksv])
        yield

    def run_gen(g, k=None):
        n_ = 0
        while g is not None and (k is None or n_ < k):
            try:
                next(g)
            except StopIteration:
                return None
            n_ += 1
        return g

    run_gen(topk_gen(0))
    for tc in range(16):
        sv = slotv[tc % 2]
        ksv = "slotv%d" % (tc % 2)
        nxt = topk_gen(tc + 1) if tc + 1 < 16 else None
        gs = tc % 2
        Gt3 = Gt[gs].rearrange("p (c t) -> p c t", c=128)
        def stA(q, tc=tc):
            tb, sg = q // 8, q % 8
            rs = tb % 2
            if sg == 0:
                for p_ in range(2):
                    for pc in range(3):
                        srcb = s3_D[tc, pc, p_]
                        src = AP(srcb.tensor, srcb.offset + (tb * 32) * 128, [[2 * 16384, 8], [1, 4096]])
                        dma(SP, (s1h, s2h)[p_][rs][pc * 8:(pc + 1) * 8, :], src, ["s3_D"], ["sh%d_%d" % (p_, rs)],
                            semkey="sh%d_%d" % (p_, rs))
            st = q % 2
            b1, b2 = 2 * st, 2 * st + 1
            mm(psf(b1), rep_b, s1h[rs][0:24, sg * 512:(sg + 1) * 512], True, True, ["rep_b", "sh0_%d" % rs], [psk[b1]])
            mm(psf(b2), rep_b, s2h[rs][0:24, sg * 512:(sg + 1) * 512], True, True, ["rep_b", "sh1_%d" % rs], [psk[b2]])

        def stB(q, sv=sv, ksv=ksv):
            gi, st, tl = q % NG, q % 2, q * 4
            b1, b2 = 2 * st, 2 * st + 1
            oh3 = ohb[gi].rearrange("p (t n) -> p t n", t=4)
            mk3 = mkb[gi].rearrange("p (t n) -> p t n", t=4)
            ee3 = eeb[gi].rearrange("p (t n) -> p t n", t=4)
            tt(DVE, oh3, psf(b1).rearrange("p (t n) -> p t n", t=4), raw(sv, tl, [[1, 4], [0, 128]]), ALU.is_equal,
               [psk[b1], ksv], ["oh%d" % gi])
            uu3 = uub[gi].rearrange("p (t n) -> p t n", t=4)
            tt(DVE, uu3, psf(b2).rearrange("p (t n) -> p t n", t=4), raw(sv, tl, [[1, 4], [0, 128]]), ALU.add,
               [psk[b2], ksv], ["uu%d" % gi])
            tt(DVE, mk3, uu3, raw(sv, 128 + tl, [[1, 4], [0, 128]]), ALU.is_ge, ["uu%d" % gi, ksv], ["mk%d" % gi])
            for t4 in range(4):
                col = tl + t4
                act(ee3[:, t4, :], psf(b2)[:, t4 * 128:(t4 + 1) * 128], AF.Exp, [psk[b2], ksv], ["ee%d" % gi],
                    bias=sv[:, 256 + col:256 + col + 1], scale=1.0)

        def stC(q):
            gi = q % NG
            tt(POOL, RRb[gi], mkb[gi], eeb[gi], ALU.mult, ["mk%d" % gi, "ee%d" % gi], ["RR%d" % gi])

        def stD(q):
            gi, bo = q % NG, 4 + q % 3
            oh3 = ohb[gi].rearrange("p (t n) -> p t n", t=4)
            RR3 = RRb[gi].rearrange("p (t n) -> p t n", t=4)
            for t4 in range(4):
                mm(psf(bo).rearrange("p (c t) -> p t c", t=4)[:, t4, :], oh3[:, t4, :], RR3[:, t4, :], True, True,
                   ["oh%d" % gi, "RR%d" % gi], [psk[bo]])

        def stE(q, Gt3=Gt3, gs=gs):
            bo, tl = 4 + q % 3, q * 4
            cp(ACT, Gt3[:, :, tl:tl + 4], psf(bo).rearrange("p (c t) -> p c t", t=4), [psk[bo]], ["Gt%d" % gs])

        NQ = 32
        for n in range(NQ + 3):
            if n < NQ:
                stA(n)
            if 0 <= n - 1 < NQ:
                stB(n - 1)
            if 0 <= n - 2 < NQ:
                stC(n - 2)
                stD(n - 2)
            if 0 <= n - 3 < NQ:
                stE(n - 3)
            nxt = run_gen(nxt, 4)
        run_gen(nxt)
        dma(POOL, G_D[tc], Gt[gs], ["Gt%d" % gs], ["G_D"], semkey="st_G")
    P.barrier()

    A = Arena()
    x2r = A.f(8192)
    x2h = x2r[:, :].bitcast(BF16).rearrange("p (k t) -> p k t", k=16)
    h1q = [x2r[:, i * 2048:(i + 1) * 2048] for i in range(4)]
    oacc = A.f(8 * 2048).rearrange("p (c m) -> p c m", c=8)
    wdr = [A.f(4096) for _ in range(2)]
    wur = [A.f(4096) for _ in range(2)]
    wd = [w_[:, :].bitcast(BF16).rearrange("p (k e) -> p k e", k=16) for w_ in wdr]
    wu = [w_[:, :].bitcast(BF16).rearrange("p (c m) -> p c m", c=4) for w_ in wur]
    GlT = [A.h(2048).rearrange("p (a c t) -> p a c t", a=4, c=4) for _ in range(3)]
    gel = [A.h(2048).rearrange("p (c t) -> p c t", c=4) for _ in range(2)]
    Am = [A.h(2048).rearrange("p (a c t) -> p a c t", a=4, c=4) for _ in range(2)]
    stat5 = [A.f(4) for _ in range(2)]
    h1l = [wdr[0][:, 0:2048], wdr[0][:, 2048:4096]]
    yyl = [wdr[1][:, 0:2048], wdr[1][:, 2048:4096]]
    oo = [wur[0][:, 0:2048], wur[0][:, 2048:4096]]
    g2g = wur[1][:, 0:2048]
    gfb = wur[1][:, 2048:4096]
    pend5 = []
    it5c = [0]

    def p5_front(H, et, tgl, i5):
        ws, gsl, s2 = et % 2, i5 % 3, i5 % 2
        for a in range(4):
            tcg = H * 8 + tgl * 4 + a
            dma(SP, GlT[gsl][:, a, :, :], G_D[tcg, :, et * 512:(et + 1) * 512].rearrange("p (c t) -> p c t", c=4),
                ["G_D"], ["GlT%d" % gsl], semkey="GlT%d" % gsl)
        for c4 in range(4):
            for k in range(16):
                mm(psf(c4), wd[ws][:, k, c4 * 128:(c4 + 1) * 128], x2h[:, k, tgl * 512:(tgl + 1) * 512], k == 0, k == 15,
                   ["x2h", "wd%d" % ws], [psk[c4]])
            act(gel[s2][:, c4, :], psf(c4), AF.Gelu, [psk[c4]], ["gel%d" % s2])
        for a in range(4):
            tt(DVE, Am[s2][:, a, :, :], gel[s2][:, :, a * 128:(a + 1) * 128], GlT[gsl][:, a, :, :], ALU.mult,
               ["gel%d" % s2, "GlT%d" % gsl], ["Am%d" % s2])

    def p5_back(H, et, tgl, i5):
        ws, s2 = et % 2, i5 % 2
        for a in range(4):
            tcl = tgl * 4 + a
            for dn in range(4):
                for c4 in range(4):
                    mm(psf(4 + dn), Am[s2][:, a, c4, :], wu[ws][:, c4, dn * 512:(dn + 1) * 512], c4 == 0, c4 == 3,
                       ["Am%d" % s2, "wu%d" % ws], [psk[4 + dn]])
            for dn in range(4):
                ov = oacc[:, tcl, dn * 512:(dn + 1) * 512]
                if et == 0:
                    cp(DVE, ov, psf(4 + dn), [psk[4 + dn]], ["oacc%d" % tcl])
                else:
                    tt(DVE, ov, ov, psf(4 + dn), ALU.add, [psk[4 + dn], "oacc%d" % tcl], ["oacc%d" % tcl])

    def p5_iter(H, et, tgl):
        i5 = it5c[0]
        it5c[0] += 1
        p5_front(H, et, tgl, i5)
        if pend5:
            p5_back(*pend5.pop())
        pend5.append((H, et, tgl, i5))

    for H in range(2):
        dma(SP, x2h, xn2T_D[:, :, H * 1024:(H + 1) * 1024].rearrange("k p t -> p k t"), ["xn2T_D"], ["x2h"])
        for et in range(32):
            ws = et % 2
            dma(POOL, wd[ws], wdT[:, et * 512:(et + 1) * 512].rearrange("(k p) e -> p k e", p=128), [], ["wd%d" % ws])
            dma(POOL, wu[ws], wup[et * 512:(et + 1) * 512, :].rearrange("(c p) m -> p c m", p=128), [], ["wu%d" % ws])
            for tgl in range(2):
                p5_iter(H, et, tgl)
        if pend5:
            p5_back(*pend5.pop())
        P.barrier()
        dma(SP, g2g, mod_D[0:1, 10240:12288].partition_broadcast(128), ["mod_D"], ["e_g2g"])
        dma(SP, gfb, gf[0:1, :].partition_broadcast(128), [], ["e_gfb"])
        junk5b = [gel[0].rearrange("p c t -> p (c t)"), gel[1].rearrange("p c t -> p (c t)")]
        def ld_h1(tcl, H=H):
            tcg = H * 8 + tcl
            dma(SP, h1q[tcl % 4], h1_D[tcg * 128:(tcg + 1) * 128, :], ["h1_D"], ["e_h1%d" % (tcl % 4)])

        for tcl in range(3):
            ld_h1(tcl)

        def ep_X(tcl, H=H):
            s = tcl % 2
            yv = yyl[s]
            if tcl + 3 < 8:
                ld_h1(tcl + 3)
            tt(DVE, yv, oacc[:, tcl, :], g2g, ALU.mult, ["oacc%d" % tcl, "e_g2g"], ["e_y%d" % s])
            tt(POOL, yv, yv, h1q[tcl % 4], ALU.add, ["e_y%d" % s, "e_h1%d" % (tcl % 4)], ["e_y%d" % s])
            ss, rt, rstd = stat5[s][:, 0:1], stat5[s][:, 1:2], stat5[s][:, 2:3]
            act(junk5b[s], yv, AF.Square, ["e_y%d" % s], ["e_j%d" % s, "e_ss%d" % s], accum=ss)
            act(rt, ss, AF.Sqrt, ["e_ss%d" % s], ["e_rt%d" % s], bias=1e-6, scale=1.0 / 2048)

        def ep_Y(tcl, H=H):
            s = tcl % 2
            yv = yyl[s]
            tcg = H * 8 + tcl
            rt, rstd = stat5[s][:, 1:2], stat5[s][:, 2:3]
            recip(rstd, rt, ["e_rt%d" % s], ["e_rs%d" % s])
            amul(yv, yv, rstd, ["e_y%d" % s, "e_rs%d" % s], ["e_y%d" % s])
            tt(DVE, oo[s], yv, gfb, ALU.mult, ["e_y%d" % s, "e_gfb"], ["e_o%d" % s])
            dma(SP, out[tcg * 128:(tcg + 1) * 128, :], oo[s], ["e_o%d" % s], ["out"], semkey="st_out%d" % s)

        ep_X(0)
        for tcl in range(8):
            if tcl + 1 < 8:
                ep_X(tcl + 1)
            ep_Y(tcl)
        P.barrier()

    P.emit()
    es.close()
    return nc


_CACHE = {}


def _host_consts():
    if "c" in _CACHE:
        return _CACHE["c"]
    ident = np.eye(128, dtype=np.float32)
    R = np.zeros((128, 128), np.float32)
    for d in range(64):
        R[d, d + 64] = -1.0
        R[d + 64, d] = 1.0
    rotT = np.ascontiguousarray(R.T)
    k = np.arange(128)[:, None]
    q = np.arange(128)[None, :]
    mprev = (k >= q).astype(np.float32)
    mcur = (k <= q).astype(np.float32)
    half = 64
    inv = np.power(np.float32(10000.0), -np.arange(half, dtype=np.float32) / np.float32(half)).astype(np.float32)
    _CACHE["c"] = dict(ident=ident, rotT=rotT, mprev=mprev, mcur=mcur, inv=inv)
    return _CACHE["c"]


def kernel(x, c, ada_w, ada_b, norm1_g, w_in, pool_mix, pool_scale, w_attn_branch, w_pool_branch, w_out,
           norm2_g, peer_wq, peer_keys, peer_down, peer_up, final_g, _debug=()):
    f32 = np.float32
    x = np.asarray(x, f32)
    hc = _host_consts()
    xs = x[0]
    shared = dict(
        c_in=np.ascontiguousarray(np.asarray(c, f32).reshape(16, 128).T),
        ada_w=np.ascontiguousarray(np.asarray(ada_w, f32)[0]),
        ada_b=np.ascontiguousarray(np.asarray(ada_b, f32).reshape(1, 12288)),
        g1=np.asarray(norm1_g, f32).reshape(1, 2048),
        g2=np.asarray(norm2_g, f32).reshape(1, 2048),
        gf=np.asarray(final_g, f32).reshape(1, 2048),
        g1T=np.ascontiguousarray(np.asarray(norm1_g, f32).reshape(16, 128).T),
        g2T=np.ascontiguousarray(np.asarray(norm2_g, f32).reshape(16, 128).T),
        w_in=np.ascontiguousarray(np.asarray(w_in, f32)[0]),
        pmix=np.ascontiguousarray(np.asarray(pool_mix, f32).reshape(512, 128)),
        pscale=np.ascontiguousarray(np.asarray(pool_scale, f32).reshape(4, 128).T),
        wab=np.ascontiguousarray(np.asarray(w_attn_branch, f32)[0]),
        wpb=np.ascontiguousarray(np.asarray(w_pool_branch, f32)[0]),
        w_out=np.ascontiguousarray(np.asarray(w_out, f32)[0]),
        wq=np.ascontiguousarray(np.asarray(peer_wq, f32)[0]),
        keysT=np.ascontiguousarray(np.asarray(peer_keys, f32)[0].reshape(16, 128, 128).transpose(2, 0, 1).reshape(128, 2048)),
        wdT=np.ascontiguousarray(np.asarray(peer_down, f32)[0].reshape(128, 128, 2048).transpose(2, 1, 0).reshape(2048, 16384)),
        wup=np.ascontiguousarray(np.asarray(peer_up, f32)[0].reshape(128, 128, 2048).transpose(1, 0, 2).reshape(16384, 2048)),
        ident=hc["ident"], rotT=hc["rotT"], repm=np.ascontiguousarray(np.tile(np.kron(np.eye(8, dtype=np.float32), np.ones((1, 16), np.float32)), (3, 1))), mprev=hc["mprev"], mcur=hc["mcur"],
    )
    in_maps = []
    for core in range(NCORES):
        base = core * 2048
        xh = np.zeros((4096, 2048), f32)
        if core > 0:
            xh[:2048] = xs[base - 2048:base]
        xh[2048:] = xs[base:base + 2048]
        pos = (np.arange(4096) + (base - 2048)).astype(f32)
        ang = (pos[:, None] * hc["inv"][None, :]).astype(f32)
        cosT = np.ascontiguousarray(np.concatenate([np.cos(ang), np.cos(ang)], axis=1).T.astype(f32))
        sinT = np.ascontiguousarray(np.concatenate([np.sin(ang), np.sin(ang)], axis=1).T.astype(f32))
        pc = np.zeros((4, 16), f32)
        for g, w in enumerate((2, 4, 8, 16)):
            for t in range(16):
                pc[g, t] = 1.0 / (min(t + 1, w) if core == 0 else w)
        m = dict(shared)
        m.update(
            xh=xh, cosT=cosT, sinT=sinT,
            mfirst=(hc["mprev"] if core > 0 else np.zeros((128, 128), f32)),
            flag=np.full((128, 1), 1.0 if core > 0 else 0.0, f32),
            pcorr=np.ascontiguousarray(np.broadcast_to(pc.reshape(1, 64), (128, 64))),
        )
        in_maps.append(m)
    key = ("nc", tuple(_debug))
    if key not in _CACHE:
        _CACHE[key] = build(debug=tuple(_debug))
    nc = _CACHE[key]
    res = run_bass_kernel_spmd(nc, in_maps, core_ids=list(range(NCORES)))
    outs = [np.asarray(r["out"], f32) for r in res.results]
    full = np.concatenate(outs, axis=0).reshape(1, 16384, 2048)
    if _debug:
        DBG["res"] = res.results
    return full
```
